# Optimizing a Trainium2 kernel written in Bass

```python
import math
import jax, jax.numpy as jnp
from jax import lax
import numpy as np

D_MODEL = 1024
BATCH = 8
SEQ = 4096
DEPTH = 2

M_HEADS = 4
M_HEAD_DIM = 128
M_WIDTH = M_HEADS * M_HEAD_DIM
M_CONV = 4
M_CHUNK = 128
M_INIT = -1e30
ROPE_DIM = 64
ROPE_THETA = 10000.0
S_HEADS = 8
S_HEAD_DIM = ROPE_DIM
S_WIDTH = S_HEADS * S_HEAD_DIM
S_KV_RANK = 256
IDX_HEADS = 4
IDX_DIM = ROPE_DIM
IDX_TOPK_MAX = 256
S_QBLOCK = 64
DA_HEADS = 4
DA_HEAD_DIM = ROPE_DIM
DA_V_DIM = 2 * DA_HEAD_DIM
DA_WIDTH = DA_HEADS * DA_V_DIM
DA_QBLOCK = 128
N_BRANCH = 3
BRANCH_WIDTH = 512
FF_DIM = ((8 * D_MODEL + 3 * 256 - 1) // (3 * 256)) * 256
EPS = 1e-6

_SEG_SIZES = (
    2 * M_WIDTH,
    M_WIDTH,
    M_WIDTH,
    2 * M_HEADS,
    S_WIDTH,
    S_KV_RANK,
    IDX_HEADS * IDX_DIM,
    IDX_DIM,
    IDX_HEADS,
    2 * DA_HEADS * DA_HEAD_DIM,
    2 * DA_HEADS * DA_HEAD_DIM,
    DA_WIDTH,
    N_BRANCH * D_MODEL,
)
IN_COLS = sum(_SEG_SIZES)

kernel_name = 'hybrid_mlstm_dsa_diffattn_gated'


def _split_cols(z):
    offs = []
    acc = 0
    for s in _SEG_SIZES[:-1]:
        acc += s
        offs.append(acc)
    return jnp.split(z, offs, axis=-1)


def _rms(x, g):
    xf = x.astype(jnp.float32)
    y = xf * lax.rsqrt(jnp.mean(xf * xf, axis=-1, keepdims=True) + EPS)
    return (y * g.astype(jnp.float32)).astype(x.dtype)


def _rope_tables(seq, dim, dtype):
    inv = 1.0 / jnp.power(ROPE_THETA, jnp.arange(0, dim, 2, dtype=jnp.float32) / dim)
    ang = jnp.arange(seq, dtype=jnp.float32)[:, None] * inv[None, :]
    return jnp.cos(ang).astype(dtype), jnp.sin(ang).astype(dtype)


def _rope(x, cos, sin):
    half = x.shape[-1] // 2
    shape = (1, cos.shape[0]) + (1,) * (x.ndim - 3) + (half,)
    c = cos.reshape(shape)
    s = sin.reshape(shape)
    x1, x2 = x[..., :half], x[..., half:]
    return jnp.concatenate([x1 * c - x2 * s, x1 * s + x2 * c], axis=-1)


def _causal_conv(x, w, b):
    k, c = w.shape
    y = lax.conv_general_dilated(x, w[:, None, :], window_strides=(1,), padding=[(k - 1, 0)],
                                 dimension_numbers=('NWC', 'WIO', 'NWC'), feature_group_count=c)
    return y + b


def _mlstm(q, k, v, i_pre, f_pre):
    B, T, H, dh = q.shape
    L = M_CHUNK
    N = T // L
    f32 = jnp.float32

    def chunk(a):
        a = a.astype(f32).reshape((B, N, L, H) + a.shape[3:])
        return jnp.moveaxis(a, 3, 1)

    qc = chunk(q)
    kc = chunk(k) * (dh ** -0.5)
    vc = chunk(v)
    ic = chunk(i_pre)
    lf = jax.nn.log_sigmoid(chunk(f_pre))
    b = jnp.cumsum(lf, axis=-1)
    g = b[..., -1]

    a = g[..., None] - b + ic
    m_loc = a.max(axis=-1)
    w_loc = jnp.exp(a - m_loc[..., None])
    c_loc = jnp.einsum('bhnlv,bhnlk->bhnvk', vc * w_loc[..., None], kc)
    n_loc = jnp.einsum('bhnl,bhnlk->bhnk', w_loc, kc)

    def step(state, xs):
        c_st, n_st, m_st = state
        g_n, c_n, nn_n, m_n = xs
        m_new = jnp.maximum(g_n + m_st, m_n)
        s_old = jnp.exp(g_n + m_st - m_new)
        s_loc = jnp.exp(m_n - m_new)
        c_new = s_old[..., None, None] * c_st + s_loc[..., None, None] * c_n
        n_new = s_old[..., None] * n_st + s_loc[..., None] * nn_n
        return (c_new, n_new, m_new), (c_st, n_st, m_st)

    init = (jnp.zeros((B, H, dh, dh), f32), jnp.zeros((B, H, dh), f32), jnp.full((B, H), M_INIT, f32))
    xs = (jnp.moveaxis(g, 2, 0), jnp.moveaxis(c_loc, 2, 0), jnp.moveaxis(n_loc, 2, 0), jnp.moveaxis(m_loc, 2, 0))
    _, (c_prev, n_prev, m_prev) = lax.scan(step, init, xs)
    c_prev = jnp.moveaxis(c_prev, 0, 2)
    n_prev = jnp.moveaxis(n_prev, 0, 2)
    m_prev = jnp.moveaxis(m_prev, 0, 2)

    t_idx = jnp.arange(L)
    causal = t_idx[:, None] >= t_idx[None, :]
    dmat = b[..., :, None] - b[..., None, :] + ic[..., None, :]
    dmat = jnp.where(causal, dmat, -jnp.inf)
    inter = b + m_prev[..., None]
    m_t = jnp.maximum(inter, dmat.max(axis=-1))
    sw = jnp.exp(dmat - m_t[..., None]) * jnp.einsum('bhntk,bhnsk->bhnts', qc, kc)
    s_inter = jnp.exp(inter - m_t)
    num = jnp.einsum('bhnts,bhnsv->bhntv', sw, vc) + s_inter[..., None] * jnp.einsum('bhntk,bhnvk->bhntv', qc, c_prev)
    den = sw.sum(axis=-1) + s_inter * jnp.einsum('bhntk,bhnk->bhnt', qc, n_prev)
    h = num / jnp.maximum(jnp.abs(den), jnp.exp(-m_t))[..., None]
    return jnp.moveaxis(h, 1, 3).reshape(B, T, H, dh)


def _dsa(q, k, v, qi, ki, wi):
    B, T, H, dh = q.shape
    topk = min(IDX_TOPK_MAX, T // 4)
    nb = T // S_QBLOCK
    f32 = jnp.float32
    bidx = jnp.arange(B)[:, None, None]
    ki32 = ki.astype(f32)
    key_pos = jnp.arange(T)
    idx_scale = (IDX_HEADS * IDX_DIM) ** -0.5

    def blocks(a):
        return jnp.moveaxis(a.reshape((B, nb, S_QBLOCK) + a.shape[2:]), 1, 0)

    def blk(args):
        qb, qib, wib, start = args
        tq = start + jnp.arange(S_QBLOCK)
        causal = key_pos[None, :] <= tq[:, None]
        rel = jax.nn.relu(jnp.einsum('bqhd,bsd->bqhs', qib.astype(f32), ki32))
        score = jnp.einsum('bqh,bqhs->bqs', wib.astype(f32) * idx_scale, rel)
        score = jnp.where(causal[None], score, -jnp.inf)
        _, idx = lax.top_k(score, topk)
        valid = idx <= tq[None, :, None]
        k_sel = k[bidx, idx]
        v_sel = v[bidx, idx]
        s = jnp.einsum('bqhd,bqkhd->bhqk', qb, k_sel).astype(f32) * (dh ** -0.5)
        s = jnp.where(valid[:, None], s, -jnp.inf)
        p = jax.nn.softmax(s, axis=-1).astype(v.dtype)
        return jnp.einsum('bhqk,bqkhd->bqhd', p, v_sel)

    starts = jnp.arange(nb, dtype=jnp.int32) * S_QBLOCK
    out = lax.map(blk, (blocks(q), blocks(qi), blocks(wi), starts))
    return jnp.moveaxis(out, 0, 1).reshape(B, T, H, dh)


def _diff_attn(q, k, v, lam):
    B, T, H, _, d = q.shape
    nb = T // DA_QBLOCK
    f32 = jnp.float32
    key_pos = jnp.arange(T)

    def blk(args):
        qb, start = args
        tq = start + jnp.arange(DA_QBLOCK)
        causal = key_pos[None, :] <= tq[:, None]
        s = jnp.einsum('bqhcd,bshcd->bhcqs', qb, k).astype(f32) * (d ** -0.5)
        s = jnp.where(causal, s, -jnp.inf)
        p = jax.nn.softmax(s, axis=-1)
        a = p[:, :, 0] - lam * p[:, :, 1]
        return jnp.einsum('bhqs,bshv->bqhv', a.astype(v.dtype), v)

    qblk = jnp.moveaxis(q.reshape((B, nb, DA_QBLOCK) + q.shape[2:]), 1, 0)
    starts = jnp.arange(nb, dtype=jnp.int32) * DA_QBLOCK
    out = lax.map(blk, (qblk, starts))
    return jnp.moveaxis(out, 0, 1).reshape(B, T, H, v.shape[-1])


def _layer(x, li, cos, sin, norm_mix_g, w_in, b_gate, conv_w, conv_b, gate_b, m_norm_g,
           kv_norm_g, w_kv_up, sq_g, sk_g, dq_g, dk_g, lam, d_out_g, w_branch, w_out,
           norm_ffn_g, w_gate_up, w_down):
    B, T, _ = x.shape
    f32 = jnp.float32
    xn = _rms(x, norm_mix_g)
    z = jnp.einsum('btd,dc->btc', xn, w_in)
    (m_qk, m_v, m_o, m_if, s_q, s_ckv, i_q, i_k, i_w, d_q, d_k, d_v, g_pre) = _split_cols(z)

    m_qk = jax.nn.silu(_causal_conv(m_qk, conv_w, conv_b))
    mq, mk = jnp.split(m_qk, 2, axis=-1)
    m_if = m_if + gate_b
    h = _mlstm(mq.reshape(B, T, M_HEADS, M_HEAD_DIM), mk.reshape(B, T, M_HEADS, M_HEAD_DIM),
               m_v.reshape(B, T, M_HEADS, M_HEAD_DIM), m_if[..., :M_HEADS], m_if[..., M_HEADS:])
    h = _rms(h, m_norm_g).reshape(B, T, M_WIDTH)
    y_a = (jax.nn.sigmoid(m_o.astype(f32)) * h).astype(x.dtype)

    sq = _rope(_rms(s_q.reshape(B, T, S_HEADS, S_HEAD_DIM), sq_g), cos, sin)
    ckv = _rms(s_ckv, kv_norm_g)
    kv = jnp.einsum('btr,rc->btc', ckv, w_kv_up).reshape(B, T, 2, S_HEADS, S_HEAD_DIM)
    sk = _rope(_rms(kv[:, :, 0], sk_g), cos, sin)
    sv = kv[:, :, 1]
    iq = _rope(i_q.reshape(B, T, IDX_HEADS, IDX_DIM), cos, sin)
    ik = _rope(i_k, cos, sin)
    y_b = _dsa(sq, sk, sv, iq, ik, i_w).reshape(B, T, S_WIDTH)

    dq = _rope(_rms(d_q.reshape(B, T, DA_HEADS, 2, DA_HEAD_DIM), dq_g), cos, sin)
    dk = _rope(_rms(d_k.reshape(B, T, DA_HEADS, 2, DA_HEAD_DIM), dk_g), cos, sin)
    dv = d_v.reshape(B, T, DA_HEADS, DA_V_DIM)
    lamf = lam.astype(f32)
    lam_init = 0.8 - 0.6 * math.exp(-0.3 * li)
    lam_val = jnp.exp(jnp.sum(lamf[0] * lamf[1])) - jnp.exp(jnp.sum(lamf[2] * lamf[3])) + lam_init
    o = _diff_attn(dq, dk, dv, lam_val)
    y_c = (_rms(o, d_out_g) * (1.0 - lam_init)).reshape(B, T, DA_WIDTH)

    gates = jax.nn.sigmoid((g_pre + b_gate).astype(f32)).astype(x.dtype).reshape(B, T, N_BRANCH, D_MODEL)
    merged = (gates[:, :, 0] * jnp.einsum('btc,cd->btd', y_a, w_branch[0])
              + gates[:, :, 1] * jnp.einsum('btc,cd->btd', y_b, w_branch[1])
              + gates[:, :, 2] * jnp.einsum('btc,cd->btd', y_c, w_branch[2]))
    x = x + jnp.einsum('btd,de->bte', merged, w_out)

    xn = _rms(x, norm_ffn_g)
    gu = jnp.einsum('btd,df->btf', xn, w_gate_up)
    g, u = jnp.split(gu, 2, axis=-1)
    return x + jnp.einsum('btf,fd->btd', jax.nn.silu(g) * u, w_down)


def setup_inputs(seed: int = 0) -> dict:
    key = jax.random.key(seed)
    ks = jax.random.split(key, 24)
    nrm = jax.random.normal
    f32 = jnp.float32

    def gain(k, shape):
        return 1.0 + 0.02 * nrm(k, shape, f32)

    i_bias = 0.1 * nrm(ks[6], (DEPTH, M_HEADS), f32)
    f_bias = jnp.linspace(3.0, 6.0, M_HEADS, dtype=f32)[None, :] + 0.1 * nrm(ks[7], (DEPTH, M_HEADS), f32)
    return {
        'x': nrm(ks[0], (BATCH, SEQ, D_MODEL), f32),
        'norm_mix_g': gain(ks[1], (DEPTH, D_MODEL)),
        'w_in': nrm(ks[2], (DEPTH, D_MODEL, IN_COLS), f32) * D_MODEL ** -0.5,
        'b_gate': 0.02 * nrm(ks[3], (DEPTH, N_BRANCH * D_MODEL), f32),
        'mlstm_conv_w': nrm(ks[4], (DEPTH, M_CONV, 2 * M_WIDTH), f32) * M_CONV ** -0.5,
        'mlstm_conv_b': 0.02 * nrm(ks[5], (DEPTH, 2 * M_WIDTH), f32),
        'mlstm_gate_b': jnp.concatenate([i_bias, f_bias], axis=-1),
        'mlstm_norm_g': gain(ks[8], (DEPTH, M_HEADS, M_HEAD_DIM)),
        'dsa_kv_norm_g': gain(ks[9], (DEPTH, S_KV_RANK)),
        'dsa_w_kv_up': nrm(ks[10], (DEPTH, S_KV_RANK, 2 * S_WIDTH), f32) * S_KV_RANK ** -0.5,
        'dsa_q_norm_g': gain(ks[11], (DEPTH, S_HEAD_DIM)),
        'dsa_k_norm_g': gain(ks[12], (DEPTH, S_HEAD_DIM)),
        'diff_q_norm_g': gain(ks[13], (DEPTH, DA_HEAD_DIM)),
        'diff_k_norm_g': gain(ks[14], (DEPTH, DA_HEAD_DIM)),
        'diff_lambda': 0.1 * nrm(ks[15], (DEPTH, 4, DA_HEAD_DIM), f32),
        'diff_out_norm_g': gain(ks[16], (DEPTH, DA_V_DIM)),
        'w_branch': nrm(ks[17], (DEPTH, N_BRANCH, BRANCH_WIDTH, D_MODEL), f32) * BRANCH_WIDTH ** -0.5,
        'w_out': nrm(ks[18], (DEPTH, D_MODEL, D_MODEL), f32) * D_MODEL ** -0.5,
        'norm_ffn_g': gain(ks[19], (DEPTH, D_MODEL)),
        'w_gate_up': nrm(ks[20], (DEPTH, D_MODEL, 2 * FF_DIM), f32) * D_MODEL ** -0.5,
        'w_down': nrm(ks[21], (DEPTH, FF_DIM, D_MODEL), f32) * FF_DIM ** -0.5,
    }


def reference(x, norm_mix_g, w_in, b_gate, mlstm_conv_w, mlstm_conv_b, mlstm_gate_b, mlstm_norm_g,
              dsa_kv_norm_g, dsa_w_kv_up, dsa_q_norm_g, dsa_k_norm_g, diff_q_norm_g, diff_k_norm_g,
              diff_lambda, diff_out_norm_g, w_branch, w_out, norm_ffn_g, w_gate_up, w_down):
    cos, sin = _rope_tables(x.shape[1], ROPE_DIM, x.dtype)
    for li in range(DEPTH):
        x = _layer(x, li, cos, sin, norm_mix_g[li], w_in[li], b_gate[li], mlstm_conv_w[li],
                   mlstm_conv_b[li], mlstm_gate_b[li], mlstm_norm_g[li], dsa_kv_norm_g[li],
                   dsa_w_kv_up[li], dsa_q_norm_g[li], dsa_k_norm_g[li], diff_q_norm_g[li],
                   diff_k_norm_g[li], diff_lambda[li], diff_out_norm_g[li], w_branch[li], w_out[li],
                   norm_ffn_g[li], w_gate_up[li], w_down[li])
    return x
```

```python
import math
from contextlib import ExitStack

import numpy as np
import concourse.bass as bass
import concourse.mybir as mybir
from concourse.bass_utils import run_bass_kernel_spmd

F32 = mybir.dt.float32
BF16 = mybir.dt.bfloat16
AF = mybir.ActivationFunctionType
ALU = mybir.AluOpType
AX = mybir.AxisListType

D = 1024
FF = 2816
IN_COLS = 7756
EPS = 1e-6
ENGS = ("pe", "act", "dve", "pool", "sp")


class Op:
    __slots__ = ("eng", "fn", "deps", "signal", "sigval", "chan", "idx", "isdma")

    def __init__(self, eng, fn, chan=None):
        self.eng = eng
        self.fn = fn
        self.deps = set()
        self.signal = False
        self.sigval = 0
        self.chan = chan
        self.isdma = chan is not None
        self.idx = -1


def _is_psum_key(k):
    if isinstance(k, tuple):
        return k[0] == "pO"
    return isinstance(k, str) and len(k) > 1 and k[0] == "p" and k != "pw2"


class Phase:
    def __init__(self, nc, name, nchan=12):
        self.nc = nc
        self.name = name
        self.ops = []
        self.last_w = {}
        self.readers = {}
        self.chan_last = {}
        self.nchan = nchan
        self.rr = 0
        self.es = ExitStack()

    def alloc(self, name, shape, dt):
        return self.es.enter_context(self.nc.sbuf_tensor(f"{self.name}_{name}", list(shape), dt))

    def psum(self, name, shape, dt=F32):
        shape = list(shape)
        n = 1
        for s_ in shape[1:]:
            n *= s_
        per_bank = 512 if dt == F32 else 1024
        nb = (n + per_bank - 1) // per_bank
        t = self.es.enter_context(self.nc.psum_tensor(f"{self.name}_{name}", [128, nb * per_bank], dt))
        v = t[:, 0:n]
        if len(shape) == 3:
            v = v.rearrange("p (a b) -> p a b", b=shape[2])
        return v

    oplimit = 10 ** 9

    def _add(self, op, reads, writes):
        if len(self.ops) >= Phase.oplimit and Phase.count + 1 == Phase.limit:
            return op
        op.idx = len(self.ops)
        deps = set()
        for r in reads:
            w = self.last_w.get(r)
            if w is not None:
                deps.add(w)
            if _is_psum_key(r):
                for rd in self.readers.get(r, ()):
                    if self.ops[rd].eng != op.eng:
                        deps.add(rd)
        for r in writes:
            w = self.last_w.get(r)
            if w is not None:
                deps.add(w)
            for rd in self.readers.get(r, ()):
                deps.add(rd)
        if op.isdma:
            prev = self.chan_last.get(op.chan)
            if prev is not None:
                deps.add(prev)
            self.chan_last[op.chan] = op.idx
        deps.discard(op.idx)
        op.deps = deps
        self.ops.append(op)
        for r in writes:
            self.last_w[r] = op.idx
            self.readers[r] = []
        for r in reads:
            if r not in writes:
                self.readers.setdefault(r, []).append(op.idx)
        return op

    def op(self, eng, fn, reads=(), writes=()):
        return self._add(Op(eng, fn), tuple(reads), tuple(writes))

    def dma(self, q, out, in_, reads=(), writes=(), slow=False):
        chan = self.rr % self.nchan
        self.rr += 1
        if slow:
            fn = lambda e: e.dma_start(out=out, in_=in_, allow_slow_non_contiguous=True)
        else:
            fn = lambda e: e.dma_start(out=out, in_=in_)
        return self._add(Op(q, fn, chan=chan), tuple(reads), tuple(writes))

    count = 0
    limit = 10 ** 9
    verbose = False
    trace = None
    gsem = None

    def emit(self):
        Phase.count += 1
        if Phase.verbose:
            print("phase", self.name, "ops", len(self.ops), "sbuf_remaining", self.nc.sbuf_bytes_remaining, flush=True)
        if Phase.count > Phase.limit:
            self.es.close()
            return 0
        nc = self.nc
        ops = self.ops
        es = self.es
        per_eng = {e: [o for o in ops if o.eng == e] for e in ENGS}
        for e in ENGS:
            comp = [o for o in per_eng[e] if not o.isdma]
            if comp:
                comp[-1].signal = True
        for o in ops:
            for d in o.deps:
                p = ops[d]
                if p.isdma:
                    continue
                if p.eng == "pe" and o.eng == "pe" and not o.isdma:
                    continue
                p.signal = True
        if Phase.gsem is None:
            Phase.gsem = ({e: nc.alloc_semaphore(f"g_e_{e}") for e in ENGS},
                          {c: nc.alloc_semaphore(f"g_c_{c}") for c in range(self.nchan)},
                          nc.alloc_semaphore("g_fin"))
            Phase.gcnt = ({e: 0 for e in ENGS}, {c: 0 for c in range(self.nchan)})
        esem, csem_all, fin = Phase.gsem
        chans = sorted({o.chan for o in ops if o.isdma})
        csem = {c: csem_all[c] for c in chans}
        fin_target = len(ENGS) * Phase.count
        cnt, ccnt = Phase.gcnt
        base = dict(cnt)
        for o in ops:
            if o.isdma:
                ccnt[o.chan] += 16
                o.sigval = ccnt[o.chan]
            elif o.signal:
                cnt[o.eng] += 1
                o.sigval = cnt[o.eng]

        def run(eng_name, e):
            waited = {}
            for o in per_eng[eng_name]:
                need = {}
                for d in o.deps:
                    p = ops[d]
                    if p.isdma:
                        key = ("c", p.chan)
                        sem = csem[p.chan]
                    else:
                        if p.eng == "pe" and o.eng == "pe" and not o.isdma:
                            continue
                        key = ("e", p.eng)
                        sem = esem[p.eng]
                    if p.sigval > need.get(key, (None, 0))[1]:
                        need[key] = (sem, p.sigval)
                for key, (sem, val) in need.items():
                    if waited.get(key, 0) >= val:
                        continue
                    e.wait_ge(sem, val)
                    waited[key] = val
                    if Phase.trace is not None:
                        Phase.trace.append((self.name, eng_name, "wait", key, val, o.idx))
                if Phase.trace is not None:
                    Phase.trace.append((self.name, eng_name, "op", o.idx, o.sigval if (o.signal or o.isdma) else None, o.chan))
                ins = o.fn(e)
                if o.isdma:
                    ins.then_inc(csem[o.chan], 16)
                elif o.signal:
                    ins.then_inc(esem[o.eng], 1)
            for c in chans:
                mine = [o for o in per_eng[eng_name] if o.isdma and o.chan == c]
                if mine and waited.get(("c", c), 0) < mine[-1].sigval:
                    e.wait_ge(csem[c], mine[-1].sigval)
            if cnt[eng_name] > base[eng_name] and waited.get(("e", eng_name), 0) < cnt[eng_name]:
                e.wait_ge(esem[eng_name], cnt[eng_name])
            e.sem_inc(fin, 1)
            e.wait_ge(fin, fin_target)

        with nc.Block() as blk:
            @blk.tensor
            def _(e):
                run("pe", e)

            @blk.scalar
            def _(e):
                run("act", e)

            @blk.vector
            def _(e):
                run("dve", e)

            @blk.gpsimd
            def _(e):
                run("pool", e)

            @blk.sync
            def _(e):
                run("sp", e)
        n = len(ops)
        self.es.close()
        return n


def build(T=4096, L=2, dbg=False, upto=99):
    NT = T // 128
    TOPK = min(256, T // 4)
    NBIS = 20
    nc = bass.Bass("TRN2", target_bir_lowering=False)
    Phase.gsem = None
    Phase.count = 0

    def din(name, shape):
        return nc.dram_tensor(name, list(shape), F32, kind="ExternalInput").ap()

    x_in = din("x", [T, D])
    norm_mix_g = din("norm_mix_g", [L, D])
    w_in = din("w_in", [L, D, IN_COLS])
    b_gate = din("b_gate", [L, 3 * D])
    conv_w = din("mlstm_conv_w", [L, 4, D])
    conv_b = din("mlstm_conv_b", [L, D])
    gate_b = din("mlstm_gate_b", [L, 8])
    mnorm_g = din("mlstm_norm_g", [L, 512])
    kvnorm_g = din("dsa_kv_norm_g", [L, 256])
    w_kv_up = din("dsa_w_kv_up", [L, 256, 1024])
    sq_g = din("dsa_q_norm_g", [L, 64])
    sk_g = din("dsa_k_norm_g", [L, 64])
    dq_g = din("diff_q_norm_g", [L, 64])
    dk_g = din("diff_k_norm_g", [L, 64])
    dlam = din("diff_lambda", [L, 256])
    dout_g = din("diff_out_norm_g", [L, 128])
    w_branch = din("w_branch", [L, 3, 512, D])
    w_out = din("w_out", [L, D, D])
    norm_ffn_g = din("norm_ffn_g", [L, D])
    w_gate_up = din("w_gate_up", [L, D, 2 * FF])
    w_down = din("w_down", [L, FF, D])
    rope_cs = din("rope_cs", [T, 64])
    out = nc.dram_tensor("out", [T, D], F32, kind="ExternalOutput").ap()

    skind = "ExternalOutput" if dbg else "Internal"

    def scr(name, shape, dt):
        return nc.dram_tensor(name, list(shape), dt, kind=skind).ap()

    xres1 = scr("xres1", [T, D], F32)
    x1 = scr("x1", [T, D], F32)
    xn2T = scr("xn2T", [D, T], BF16)
    mqkT = scr("mqkT", [1024, T], BF16)
    mv = scr("mv", [T, 512], BF16)
    mo = scr("mo", [T, 512], BF16)
    mif = scr("mif", [T, 8], F32)
    sqT = scr("sqT", [512, T], BF16)
    skT = scr("skT", [512, T], BF16)
    sv = scr("sv", [T, 512], BF16)
    iqT = scr("iqT", [256, T], F32)
    ikT = scr("ikT", [64, T], F32)
    iw = scr("iw", [T, 4], F32)
    dqT = scr("dqT", [512, T], BF16)
    dkT = scr("dkT", [512, T], BF16)
    dv = scr("dv", [T, 512], BF16)
    gates = scr("gates", [3 * D, T], BF16)
    yaT = scr("yaT", [512, T], BF16)
    ybT = scr("ybT", [512, T], BF16)
    ycT = scr("ycT", [512, T], BF16)

    top = ExitStack()

    def galloc(name, shape, dt):
        return top.enter_context(nc.sbuf_tensor(name, list(shape), dt))

    ident_f = galloc("ident_f", [128, 128], F32)
    ident_b = galloc("ident_b", [128, 128], BF16)
    tri_f = galloc("tri_f", [128, 128], F32)
    tri_b = galloc("tri_b", [128, 128], BF16)
    ones_f = galloc("ones_f", [128, 128], F32)
    mb_b = galloc("mb_b", [128, 128], BF16)
    mbq_f = galloc("mbq_f", [128, 128], F32)
    cs_all = galloc("cs_all", [128, NT, 64], F32)
    zero_c = galloc("zero_c", [128, 1], F32)
    one_c = galloc("one_c", [128, 1], F32)
    eps_c = galloc("eps_c", [128, 1], F32)

    ph = Phase(nc, "s0")
    tmpz = ph.alloc("tmpz", [128, 128], F32)
    ph.op("pool", lambda e: e.memset(ones_f[:], 1.0), writes=["ones"])
    ph.op("pool", lambda e: e.memset(tmpz[:], 0.0), writes=["tmpz"])
    ph.op("pool", lambda e: e.memset(zero_c[:], 0.0), writes=["zc"])
    ph.op("pool", lambda e: e.memset(one_c[:], 1.0), writes=["oc"])
    ph.op("pool", lambda e: e.memset(eps_c[:], EPS), writes=["ec"])
    ph.op("pool", lambda e: e.affine_select(out=ident_f[:], in_=ones_f[:], pattern=[[-1, 128]],
                                            compare_op=ALU.is_equal, fill=0.0, base=0, channel_multiplier=1),
          reads=["ones"], writes=["identf"])
    ph.op("pool", lambda e: e.affine_select(out=tri_f[:], in_=ones_f[:], pattern=[[1, 128]],
                                            compare_op=ALU.is_ge, fill=0.0, base=0, channel_multiplier=-1),
          reads=["ones"], writes=["trif"])
    ph.op("pool", lambda e: e.affine_select(out=mb_b[:], in_=tmpz[:], pattern=[[1, 128]],
                                            compare_op=ALU.is_ge, fill=-30000.0, base=0, channel_multiplier=-1),
          reads=["tmpz"], writes=["mbb"])
    ph.op("pool", lambda e: e.affine_select(out=mbq_f[:], in_=tmpz[:], pattern=[[-1, 128]],
                                            compare_op=ALU.is_ge, fill=-1e30, base=0, channel_multiplier=1),
          reads=["tmpz"], writes=["mbq"])
    ph.op("dve", lambda e: e.tensor_copy(out=ident_b[:], in_=ident_f[:]), reads=["identf"], writes=["identb"])
    ph.op("dve", lambda e: e.tensor_copy(out=tri_b[:], in_=tri_f[:]), reads=["trif"], writes=["trib"])
    ph.dma("sp", cs_all[:], rope_cs.rearrange("(n p) c -> p n c", p=128), writes=["cs"])
    ph.emit()

    def rope(ph, tag, xin, o, H, i, rkey, wkey, ts=0):
        c = cs_all[:, i, 0:32].unsqueeze(1).to_broadcast([128, H, 32])
        s = cs_all[:, i, 32:64].unsqueeze(1).to_broadcast([128, H, 32])
        tmp = ph.ropetmp[ts]
        t1, t2, t3, t4 = (tmp[k][:, 0:H, :] for k in range(4))
        R1, R2, R3, R4 = ("rt%d_%d" % (k, ts) for k in range(1, 5))
        x1a, x2a = xin[:, :, 0:32], xin[:, :, 32:64]
        ph.op("dve", lambda e: e.tensor_tensor(out=t1, in0=x1a, in1=c, op=ALU.mult), reads=[rkey], writes=[R1])
        ph.op("dve", lambda e: e.tensor_tensor(out=t2, in0=x2a, in1=s, op=ALU.mult), reads=[rkey], writes=[R2])
        ph.op("dve", lambda e: e.tensor_tensor(out=t3, in0=x1a, in1=s, op=ALU.mult), reads=[rkey], writes=[R3])
        ph.op("dve", lambda e: e.tensor_tensor(out=t4, in0=x2a, in1=c, op=ALU.mult), reads=[rkey], writes=[R4])
        ph.op("pool", lambda e: e.tensor_tensor(out=o[:, :, 0:32], in0=t1, in1=t2, op=ALU.subtract),
              reads=[R1, R2], writes=[wkey + "a"])
        ph.op("pool", lambda e: e.tensor_tensor(out=o[:, :, 32:64], in0=t3, in1=t4, op=ALU.add),
              reads=[R3, R4], writes=[wkey + "b"])

    def load_vec_cols(ph, tile_ap, vec_ap, key):
        ph.dma("sp", tile_ap, vec_ap.rearrange("(c p) -> p c", p=128), writes=[key], slow=True)

    def bcast_rows(ph, tile_ap, vec_ap, key):
        n = vec_ap.shape[0]
        ph.dma("sp", tile_ap, vec_ap.unsqueeze(0).to_broadcast([128, n]), writes=[key])

    for li in range(L):
        if li >= upto:
            break
        xsrc = x_in if li == 0 else xres1
        xdst = out if li == L - 1 else xres1
        lam_init = 0.8 - 0.6 * math.exp(-0.3 * li)
        lay = ExitStack()
        xnT = lay.enter_context(nc.sbuf_tensor(f"xnT{li}", [128, 8, T], BF16))
        gmix = lay.enter_context(nc.sbuf_tensor(f"gmix{li}", [128, 8], F32))

        ph = Phase(nc, f"a1_{li}")
        ph.ropetmp = [[ph.alloc(f"rt{k}", [128, 8, 32], F32) for k in range(4)]]
        widx = ph.alloc("widx", [128, 8, 324], F32)
        load_vec_cols(ph, gmix[:], norm_mix_g[li], "gmix")
        ph.dma("sp", widx[:], w_in[li][:, 2824:3148].rearrange("(c p) n -> p c n", p=128), writes=["widx"])
        ph.op("dve", lambda e: e.tensor_tensor(out=widx[:], in0=widx[:],
                                               in1=gmix[:].unsqueeze(2).to_broadcast([128, 8, 324]), op=ALU.mult),
              reads=["widx", "gmix"], writes=["widx"])
        xt = [ph.alloc(f"xt{k}", [128, D], F32) for k in range(2)]
        junk = ph.alloc("junk", [128, D], BF16)
        ss = [ph.alloc(f"ss{k}", [128, 1], F32) for k in range(2)]
        xh = [ph.alloc(f"xh{k}", [128, D], F32) for k in range(2)]
        xhT = [ph.alloc(f"xhT{k}", [128, 8, 128], F32) for k in range(2)]
        zi = [ph.alloc(f"zi{k}", [128, 324], F32) for k in range(2)]
        zr = [ph.alloc(f"zr{k}", [128, 5, 64], F32) for k in range(2)]
        zT = [ph.alloc(f"zT{k}", [128, 3, 128], F32) for k in range(2)]
        iwt = [ph.alloc(f"iwt{k}", [128, 4], F32) for k in range(2)]
        pT = [ph.psum(f"pT{k}", [128, D], F32) for k in range(2)]
        pI = [ph.psum(f"pI{k}", [128, 324], F32) for k in range(2)]
        pZ = ph.psum("pZ", [128, 3, 128], F32)
        for i in range(NT):
            k = i % 2
            K = str(k)
            tsl = slice(i * 128, (i + 1) * 128)
            ph.dma("sp", xt[k][:], xsrc[tsl, :], writes=["xt" + K])
            ph.op("act", lambda e, k=k: e.activation(out=junk[:], in_=xt[k][:], func=AF.Square, accum_out=ss[k][:]),
                  reads=["xt" + K], writes=["junk", "ss" + K])
            ph.op("act", lambda e, k=k: e.activation(out=ss[k][:], in_=ss[k][:], func=AF.Sqrt, bias=eps_c[:, 0:1], scale=1.0 / D),
                  reads=["ss" + K], writes=["ss" + K])
            ph.op("dve", lambda e, k=k: e.reciprocal(out=ss[k][:], in_=ss[k][:]), reads=["ss" + K], writes=["ss" + K])
            ph.op("dve", lambda e, k=k: e.tensor_scalar(out=xh[k][:], in0=xt[k][:], scalar1=ss[k][:, 0:1], scalar2=None,
                                                        op0=ALU.mult), reads=["xt" + K, "ss" + K], writes=["xh" + K])
            for c in range(8):
                ph.op("pe", lambda e, k=k, c=c: e.transpose(pT[k][:, c * 128:(c + 1) * 128], xh[k][:, c * 128:(c + 1) * 128],
                                                            ident_f[:]), reads=["xh" + K], writes=["pT" + K])
            ph.op("act", lambda e, k=k, tsl=tsl: e.activation(out=xnT[:, :, tsl], in_=pT[k][:].rearrange("p (c t) -> p c t", c=8),
                                                              func=AF.Copy), reads=["pT" + K], writes=[("xnT", i)])
            for hb2 in range(2):
                ph.op("dve", lambda e, k=k, hb2=hb2: e.tensor_copy(out=xhT[k][:, hb2 * 4:(hb2 + 1) * 4, :],
                                                                  in_=pT[k][:, hb2 * 512:(hb2 + 1) * 512].rearrange("p (c t) -> p c t", c=4)),
                      reads=["pT" + K], writes=["xhT" + K])
            for c in range(8):
                ph.op("pe", lambda e, k=k, c=c: e.matmul(pI[k][:], lhsT=xhT[k][:, c, :], rhs=widx[:, c, :],
                                                         start=(c == 0), stop=(c == 7)),
                      reads=["xhT" + K, "widx"], writes=["pI" + K])
            ph.op("act", lambda e, k=k: e.activation(out=zi[k][:], in_=pI[k][:], func=AF.Copy), reads=["pI" + K], writes=["zi" + K])
            rope(ph, "i", zi[k][:, 0:320].rearrange("p (h d) -> p h d", d=64), zr[k][:], 5, i, "zi" + K, "zr" + K)
            ph.op("pool", lambda e, k=k: e.tensor_scalar(out=iwt[k][:], in0=zi[k][:, 320:324], scalar1=1.0 / 16.0, scalar2=None,
                                                         op0=ALU.mult), reads=["zi" + K], writes=["iwt" + K])
            ph.dma("pool", iw[tsl, :], iwt[k][:], reads=["iwt" + K], writes=[("iw", i)])
            zr2 = zr[k][:].rearrange("p h d -> p (h d)")
            for c in range(2):
                ph.op("pe", lambda e, c=c, zr2=zr2: e.transpose(pZ[:, c, :], zr2[:, c * 128:(c + 1) * 128], ident_f[:]),
                      reads=["zr" + K + "a", "zr" + K + "b"], writes=["pZ"])
            ph.op("pe", lambda e, zr2=zr2: e.transpose(pZ[0:64, 2, :], zr2[:, 256:320], ident_f[:]),
                  reads=["zr" + K + "a", "zr" + K + "b"], writes=["pZ"])
            ph.op("act", lambda e, k=k: e.activation(out=zT[k][:, 0:2, :], in_=pZ[:, 0:2, :], func=AF.Copy),
                  reads=["pZ"], writes=["zTa" + K])
            ph.op("dve", lambda e, k=k: e.tensor_copy(out=zT[k][0:64, 2, :], in_=pZ[0:64, 2, :]), reads=["pZ"], writes=["zTb" + K])
            ph.dma("act", iqT[:, tsl].rearrange("(c p) t -> p c t", p=128), zT[k][:, 0:2, :], reads=["zTa" + K], writes=[("iqT", i)])
            ph.dma("pool", ikT[:, tsl], zT[k][0:64, 2, :], reads=["zTb" + K], writes=[("ikT", i)])
        ph.emit()

        ph = Phase(nc, f"a2_{li}")
        ph.ropetmp = [[ph.alloc(f"rt{k}_{v}", [128, 8, 32], F32) for k in range(4)] for v in range(2)]
        wst = [ph.alloc(f"wst{k}", [128, 8, 512], F32) for k in range(2)]
        wb = [ph.alloc(f"wb{k}", [128, 8, 512], BF16) for k in range(2)]
        wkvs = ph.alloc("wkvs", [128, 2, 1024], F32)
        wkv = ph.alloc("wkv", [128, 2, 1024], BF16)
        kvg = ph.alloc("kvg", [128, 2], F32)
        gtile = {}
        for nm, src, sc in (("sq", sq_g, 0.125), ("sk", sk_g, 1.0), ("dq", dq_g, 0.125), ("dk", dk_g, 1.0)):
            gtile[nm] = ph.alloc("g_" + nm, [128, 64], F32)
            bcast_rows(ph, gtile[nm][:], src[li], "g_" + nm)
            if sc != 1.0:
                ph.op("pool", lambda e, t=gtile[nm], sc=sc: e.tensor_scalar(out=t[:], in0=t[:], scalar1=sc, scalar2=None, op0=ALU.mult),
                      reads=["g_" + nm], writes=["g_" + nm])
        gbt = ph.alloc("gbt", [128, 8], F32)
        bcast_rows(ph, gbt[:], gate_b[li], "gbt")
        load_vec_cols(ph, kvg[:], kvnorm_g[li], "kvg")
        ph.dma("sp", wkvs[:], w_kv_up[li].rearrange("(c p) n -> p c n", p=128), writes=["wkvs"])
        ph.op("dve", lambda e: e.tensor_tensor(out=wkv[:], in0=wkvs[:], in1=kvg[:].unsqueeze(2).to_broadcast([128, 2, 1024]),
                                               op=ALU.mult), reads=["wkvs", "kvg"], writes=["wkv"])
        pa = [ph.psum(f"pa{k}", [128, 512], F32) for k in range(2)]
        pkv = [ph.psum(f"pkv{k}", [128, 512], F32) for k in range(2)]
        pTb = ph.psum("pTb", [128, 4, 128], BF16)
        pC2 = ph.psum("pC2", [128, 2, 128], BF16)
        ob = [ph.alloc(f"ob{k}", [128, 512], BF16) for k in range(2)]
        sq2s = [ph.alloc(f"sq2{v}", [128, 512], F32) for v in range(2)]
        ssqs = [ph.alloc(f"ssq{v}", [128, 8], F32) for v in range(2)]
        xn_s = [ph.alloc(f"xn_{v}", [128, 8, 64], F32) for v in range(2)]
        xrs = [ph.alloc(f"xr{v}", [128, 8, 64], BF16) for v in range(2)]
        sq2 = sq2s[0]
        oT = [ph.alloc(f"oT{k}", [128, 4, 128], BF16) for k in range(2)]
        s1 = ph.alloc("s1", [128, 1], F32)
        ckn = ph.alloc("ckn", [128, 256], BF16)
        ckT = ph.alloc("ckT", [128, 2, 128], BF16)
        zf = [ph.alloc(f"zf{k}", [128, 8], F32) for k in range(2)]
        ef = ph.alloc("ef", [128, 4], F32)
        cnt_hn = [0]

        def headnorm_rope_T(psrc, pkey, gname, dstT, i):
            tsl = slice(i * 128, (i + 1) * 128)
            k = cnt_hn[0] % 2
            cnt_hn[0] += 1
            sq2v, ssq, xn_, xr = sq2s[k], ssqs[k], xn_s[k], xrs[k]
            SQ, SS, XN, XR = "sq2%d" % k, "ssq%d" % k, "xn_%d" % k, "xr%d" % k
            ph.op("act", lambda e: e.activation(out=sq2v[:], in_=psrc, func=AF.Square), reads=[pkey], writes=[SQ])
            ph.op("dve", lambda e: e.tensor_reduce(out=ssq[:], in_=sq2v[:].rearrange("p (h d) -> p h d", d=64), axis=AX.X, op=ALU.add),
                  reads=[SQ], writes=[SS])
            ph.op("act", lambda e: e.activation(out=ssq[:], in_=ssq[:], func=AF.Sqrt, bias=eps_c[:, 0:1], scale=1.0 / 64), reads=[SS], writes=[SS])
            ph.op("dve", lambda e: e.reciprocal(out=ssq[:], in_=ssq[:]), reads=[SS], writes=[SS])
            ph.op("dve", lambda e: e.tensor_tensor(out=xn_[:], in0=psrc.rearrange("p (h d) -> p h d", d=64),
                                                   in1=ssq[:].unsqueeze(2).to_broadcast([128, 8, 64]), op=ALU.mult),
                  reads=[pkey, SS], writes=[XN])
            ph.op("pool", lambda e: e.tensor_tensor(out=xn_[:], in0=xn_[:], in1=gtile[gname][:].unsqueeze(1).to_broadcast([128, 8, 64]),
                                                    op=ALU.mult), reads=[XN, "g_" + gname], writes=[XN])
            rope(ph, gname, xn_[:], xr[:], 8, i, XN, XR, ts=k)
            xr2 = xr[:].rearrange("p h d -> p (h d)")
            for c in range(4):
                ph.op("pe", lambda e, c=c: e.transpose(pTb[:, c, :], xr2[:, c * 128:(c + 1) * 128], ident_b[:]),
                      reads=[XR + "a", XR + "b"], writes=["pTb"])
            ph.op("act", lambda e, k=k: e.activation(out=oT[k][:], in_=pTb[:], func=AF.Copy), reads=["pTb"], writes=["oT%d" % k])
            ph.dma("act", dstT[:, tsl].rearrange("(c p) t -> p c t", p=128), oT[k][:], reads=["oT%d" % k], writes=[("dstT", gname, i)])

        blocks = [("mv", 1024, 512), ("mo", 1536, 512), ("mif", 2048, 8), ("sq", 2056, 512), ("ckv", 2568, 256),
                  ("dq", 3148, 512), ("dk", 3660, 512), ("dv", 4172, 512)]
        cnt_ob = 0
        for bi, (bn, c0, n) in enumerate(blocks):
            w = bi % 2
            W = str(w)
            for hh in range(2):
                ph.dma("sp", wst[w][:, hh * 4:(hh + 1) * 4, 0:n],
                       w_in[li][hh * 512:(hh + 1) * 512, c0:c0 + n].rearrange("(c p) n -> p c n", p=128),
                       writes=["wst" + W + str(hh)])
            ph.op("dve" if bi % 2 == 0 else "pool",
                  lambda e, w=w, n=n: e.tensor_tensor(out=wb[w][:, :, 0:n], in0=wst[w][:, :, 0:n],
                                                      in1=gmix[:].unsqueeze(2).to_broadcast([128, 8, n]), op=ALU.mult),
                  reads=["wst" + W + "0", "wst" + W + "1", "gmix"], writes=["wb" + W])
            for i in range(NT):
                tsl = slice(i * 128, (i + 1) * 128)
                p = i % 2
                P = "pa%d" % p
                for c in range(8):
                    ph.op("pe", lambda e, w=w, n=n, p=p, c=c, tsl=tsl: e.matmul(pa[p][:, 0:n], lhsT=xnT[:, c, tsl], rhs=wb[w][:, c, 0:n],
                                                                                start=(c == 0), stop=(c == 7)),
                          reads=["wb" + W], writes=[P])
                if bn in ("mv", "mo", "dv"):
                    o = cnt_ob % 2
                    cnt_ob += 1
                    fn = AF.Sigmoid if bn == "mo" else AF.Copy
                    dst = {"mv": mv, "mo": mo, "dv": dv}[bn]
                    ph.op("act", lambda e, o=o, p=p, fn=fn: e.activation(out=ob[o][:], in_=pa[p][:], func=fn), reads=[P], writes=["ob%d" % o])
                    ph.dma("act", dst[tsl, :], ob[o][:], reads=["ob%d" % o], writes=[(bn, i)])
                elif bn in ("sq", "dq", "dk"):
                    headnorm_rope_T(pa[p][:], P, bn, {"sq": sqT, "dq": dqT, "dk": dkT}[bn], i)
                elif bn == "mif":
                    z = i % 2
                    Z = "zf%d" % z
                    ph.op("dve", lambda e, z=z, p=p: e.tensor_tensor(out=zf[z][:], in0=pa[p][:, 0:8], in1=gbt[:], op=ALU.add),
                          reads=[P, "gbt"], writes=[Z])
                    ph.op("act", lambda e, z=z: e.activation(out=ef[:], in_=zf[z][:, 4:8], func=AF.Exp, scale=-1.0), reads=[Z], writes=["ef"])
                    ph.op("act", lambda e: e.activation(out=ef[:], in_=ef[:], func=AF.Ln, bias=one_c[:, 0:1]), reads=["ef"], writes=["ef"])
                    ph.op("dve", lambda e, z=z: e.tensor_scalar(out=zf[z][:, 4:8], in0=ef[:], scalar1=-1.0, scalar2=None, op0=ALU.mult),
                          reads=["ef", Z], writes=[Z])
                    ph.dma("pool", mif[tsl, :], zf[z][:], reads=[Z], writes=[("mif", i)])
                elif bn == "ckv":
                    ph.op("act", lambda e, p=p: e.activation(out=sq2[:, 0:256], in_=pa[p][:, 0:256], func=AF.Square, accum_out=s1[:]),
                          reads=[P], writes=["sq20", "s1"])
                    ph.op("act", lambda e: e.activation(out=s1[:], in_=s1[:], func=AF.Sqrt, bias=eps_c[:, 0:1], scale=1.0 / 256), reads=["s1"], writes=["s1"])
                    ph.op("dve", lambda e: e.reciprocal(out=s1[:], in_=s1[:]), reads=["s1"], writes=["s1"])
                    ph.op("dve", lambda e, p=p: e.tensor_scalar(out=ckn[:], in0=pa[p][:, 0:256], scalar1=s1[:, 0:1], scalar2=None, op0=ALU.mult),
                          reads=[P, "s1"], writes=["ckn"])
                    for c in range(2):
                        ph.op("pe", lambda e, c=c: e.transpose(pC2[:, c, :], ckn[:, c * 128:(c + 1) * 128], ident_b[:]), reads=["ckn"], writes=["pC2"])
                    ph.op("act", lambda e: e.activation(out=ckT[:], in_=pC2[:], func=AF.Copy), reads=["pC2"], writes=["ckT"])
                    for hf in range(2):
                        for c in range(2):
                            ph.op("pe", lambda e, hf=hf, c=c: e.matmul(pkv[hf][:], lhsT=ckT[:, c, :], rhs=wkv[:, c, hf * 512:(hf + 1) * 512],
                                                                       start=(c == 0), stop=(c == 1)),
                                  reads=["ckT", "wkv"], writes=["pkv%d" % hf])
                    headnorm_rope_T(pkv[0][:], "pkv0", "sk", skT, i)
                    o = cnt_ob % 2
                    cnt_ob += 1
                    ph.op("act", lambda e, o=o: e.activation(out=ob[o][:], in_=pkv[1][:], func=AF.Copy), reads=["pkv1"], writes=["ob%d" % o])
                    ph.dma("act", sv[tsl, :], ob[o][:], reads=["ob%d" % o], writes=[("sv", i)])
        ph.emit()

        ph = Phase(nc, f"a3_{li}")
        w3s = [ph.alloc(f"w3s{k}", [128, 8, 128], F32) for k in range(2)]
        w3 = [ph.alloc(f"w3{k}", [128, 8, 128], BF16) for k in range(2)]
        cw = ph.alloc("cw", [128, 8, 4], F32)
        cb = ph.alloc("cb", [128, 8], F32)
        bg = ph.alloc("bg", [128, 24], F32)
        for j in range(4):
            ph.dma("sp", cw[:, :, j], conv_w[li, j].rearrange("(c p) -> p c", p=128), reads=["cwx"] if j else [], writes=["cw"], slow=True)
        load_vec_cols(ph, cb[:], conv_b[li], "cb")
        load_vec_cols(ph, bg[:], b_gate[li], "bg")
        zc = [ph.alloc(f"zc{k}", [128, T + 3], F32) for k in range(2)]
        acc = ph.alloc("acc", [128, T], F32)
        o3 = [ph.alloc(f"o3{k}", [128, T], BF16) for k in range(2)]
        p3 = [ph.psum(f"p3{k}", [128, 512], F32) for k in range(2)]
        for k in range(2):
            ph.op("pool", lambda e, k=k: e.memset(zc[k][:, 0:3], 0.0), writes=["zc%d" % k])
        NTB = T // 512
        for fc in range(32):
            w = fc % 2
            W = str(w)
            col0 = fc * 128 if fc < 8 else 4684 + (fc - 8) * 128
            ph.dma("sp", w3s[w][:], w_in[li][:, col0:col0 + 128].rearrange("(c p) n -> p c n", p=128), writes=["w3s" + W])
            ph.op("dve" if fc % 2 == 0 else "pool",
                  lambda e, w=w: e.tensor_tensor(out=w3[w][:], in0=w3s[w][:], in1=gmix[:].unsqueeze(2).to_broadcast([128, 8, 128]), op=ALU.mult),
                  reads=["w3s" + W, "gmix"], writes=["w3" + W])
            for tb in range(NTB):
                p = (fc * NTB + tb) % 2
                P = "p3%d" % p
                bsl = slice(tb * 512, (tb + 1) * 512)
                for c in range(8):
                    ph.op("pe", lambda e, w=w, p=p, c=c, bsl=bsl: e.matmul(p3[p][:], lhsT=w3[w][:, c, :], rhs=xnT[:, c, bsl],
                                                                           start=(c == 0), stop=(c == 7)),
                          reads=["w3" + W], writes=[P])
                if fc >= 8:
                    ph.op("act", lambda e, w=w, p=p, bsl=bsl, fc=fc: e.activation(out=o3[w][:, bsl], in_=p3[p][:], func=AF.Sigmoid,
                                                                                  bias=bg[:, fc - 8:fc - 7]),
                          reads=[P, "bg"], writes=["o3" + W])
                else:
                    ph.op("act", lambda e, w=w, p=p, tb=tb: e.activation(out=zc[w][:, 3 + tb * 512:3 + (tb + 1) * 512], in_=p3[p][:], func=AF.Copy),
                          reads=[P], writes=["zc" + W])
            if fc >= 8:
                ph.dma("act", gates[(fc - 8) * 128:(fc - 7) * 128, :], o3[w][:], reads=["o3" + W], writes=[("gates", fc)])
            else:
                ph.op("dve", lambda e, w=w, fc=fc: e.tensor_scalar(out=acc[:], in0=zc[w][:, 0:T], scalar1=cw[:, fc, 0:1], scalar2=None, op0=ALU.mult),
                      reads=["zc" + W, "cw"], writes=["acc"])
                for j in range(1, 4):
                    ph.op("dve", lambda e, w=w, fc=fc, j=j: e.scalar_tensor_tensor(out=acc[:], in0=zc[w][:, j:j + T], scalar=cw[:, fc, j:j + 1],
                                                                                   in1=acc[:], op0=ALU.mult, op1=ALU.add),
                          reads=["zc" + W, "cw", "acc"], writes=["acc"])
                ph.op("act", lambda e, w=w, fc=fc: e.activation(out=o3[w][:], in_=acc[:], func=AF.Silu, bias=cb[:, fc:fc + 1]),
                      reads=["acc", "cb"], writes=["o3" + W])
                ph.dma("act", mqkT[fc * 128:(fc + 1) * 128, :], o3[w][:], reads=["o3" + W], writes=[("mqkT", fc)])
        ph.emit()
        lay.close()
        if li * 10 + 1 >= upto * 10 + (upto % 1):
            pass

        ph = Phase(nc, f"b1_{li}")
        qk = [ph.alloc(f"qk{k}", [128, 8, 128], BF16) for k in range(2)]
        vp = [ph.alloc(f"vp{k}", [128, 4, 129], BF16) for k in range(2)]
        mft = [ph.alloc(f"mft{k}", [128, 8], F32) for k in range(2)]
        mot = [ph.alloc(f"mot{k}", [128, 512], BF16) for k in range(2)]
        mg = ph.alloc("mg", [128, 512], F32)
        bcast_rows(ph, mg[:], mnorm_g[li], "mg")
        GO = ph.alloc("GO", [128, 512], F32)
        bs = ph.alloc("bs", [128, 8], F32)
        ebt = ph.alloc("ebt", [128, 4], F32)
        apre = ph.alloc("apre", [128, 4], F32)
        a_s = ph.alloc("a_s", [128, 4], F32)
        wpre = ph.alloc("wpre", [128, 4], F32)
        wstt = ph.alloc("wstt", [128, 4], F32)
        eg = ph.alloc("eg", [128, 4], F32)
        lnsc = ph.alloc("lnsc", [128, 1], F32)
        ph.op("pool", lambda e: e.memset(lnsc[:], -0.5 * math.log(128.0)), writes=["lnsc"])
        epsc = ph.alloc("epsc", [128, 1], F32)
        ph.op("pool", lambda e: e.memset(epsc[:], EPS), writes=["epsc"])
        ST = [ph.alloc(f"ST{k}", [128, 128], BF16) for k in range(2)]
        kw = [ph.alloc(f"kw{k}", [128, 128], BF16) for k in range(2)]
        CTf = [ph.alloc(f"CTf{h}", [128, 129], F32) for h in range(4)]
        CTb = [ph.alloc(f"CTb{h}", [128, 129], BF16) for h in range(4)]
        dns = [ph.alloc(f"dn{h}", [128, 1], F32) for h in range(4)]
        facs = [ph.alloc(f"fac{h}", [128, 1], F32) for h in range(4)]
        ss1s = [ph.alloc(f"ss1{h}", [128, 1], F32) for h in range(4)]
        junk1s = [ph.alloc(f"junk1{h}", [128, 128], BF16) for h in range(4)]
        ya = [ph.alloc(f"ya{k}", [128, 512], BF16) for k in range(2)]
        yT = [ph.alloc(f"yT{k}", [128, 4, 128], BF16) for k in range(2)]
        pB = ph.psum("pB", [128, 8], F32)
        pS = [ph.psum(f"pS{k}", [128, 128], F32) for k in range(2)]
        pK = ph.psum("pK", [128, 128], BF16)
        pO = [ph.psum(f"pO{k}", [128, 129], F32) for k in range(2)]
        pC = ph.psum("pC", [128, 129], F32)
        pY = ph.psum("pY", [128, 4, 128], BF16)
        for k in range(2):
            ph.op("pool", lambda e, k=k: e.memset(vp[k][:], 1.0), writes=["vp%d" % k])
        for h in range(4):
            ph.op("pool", lambda e, h=h: e.memset(CTf[h][:], 0.0), writes=["CTf%d" % h])
            ph.op("pool", lambda e, h=h: e.memset(CTb[h][:], 0.0), writes=["CTb%d" % h])
        for n in range(NT):
            k = n % 2
            K = str(k)
            tsl = slice(n * 128, (n + 1) * 128)
            ph.dma("sp", qk[k][:], mqkT[:, tsl].rearrange("(g p) t -> p g t", p=128), writes=["qk" + K])
            ph.dma("sp", vp[k][:, :, 0:128], mv[tsl, :].rearrange("t (h v) -> t h v", v=128), writes=["vp" + K])
            ph.dma("sp", mft[k][:], mif[tsl, :], writes=["mft" + K])
            ph.dma("sp", mot[k][:], mo[tsl, :], writes=["mot" + K])
            ph.op("pe", lambda e, k=k: e.matmul(pB[:, 0:4], lhsT=tri_f[:], rhs=mft[k][:, 4:8], start=True, stop=False, skip_group_check=True),
                  reads=["mft" + K], writes=["pB"])
            ph.op("pe", lambda e, k=k: e.matmul(pB[:, 4:8], lhsT=ones_f[:], rhs=mft[k][:, 4:8], start=False, stop=True, skip_group_check=True),
                  reads=["mft" + K], writes=["pB"])
            ph.op("dve", lambda e: e.tensor_copy(out=bs[:], in_=pB[:]), reads=["pB"], writes=["bs"])
            ph.op("act", lambda e: e.activation(out=ebt[:], in_=bs[:, 0:4], func=AF.Exp), reads=["bs"], writes=["ebt"])
            ph.op("act", lambda e: e.activation(out=eg[:], in_=bs[:, 4:8], func=AF.Exp), reads=["bs"], writes=["eg"])
            ph.op("dve", lambda e, k=k: e.tensor_tensor(out=apre[:], in0=mft[k][:, 0:4], in1=bs[:, 0:4], op=ALU.subtract),
                  reads=["mft" + K, "bs"], writes=["apre"])
            ph.op("act", lambda e: e.activation(out=a_s[:], in_=apre[:], func=AF.Exp, bias=lnsc[:, 0:1]), reads=["apre", "lnsc"], writes=["a_s"])
            ph.op("dve", lambda e: e.tensor_tensor(out=wpre[:], in0=apre[:], in1=bs[:, 4:8], op=ALU.add), reads=["apre", "bs"], writes=["wpre"])
            ph.op("act", lambda e: e.activation(out=wstt[:], in_=wpre[:], func=AF.Exp, bias=lnsc[:, 0:1]), reads=["wpre", "lnsc"], writes=["wstt"])
            ph.op("pool", lambda e, k=k: e.tensor_tensor(out=GO[:], in0=mot[k][:], in1=mg[:], op=ALU.mult), reads=["mot" + K, "mg"], writes=["GO"])
            y = n % 2
            for h in range(4):
                s = (n * 4 + h) % 2
                S = str(s)
                ph.op("pe", lambda e, k=k, h=h, s=s: e.matmul(pS[s][:], lhsT=qk[k][:, 4 + h, :], rhs=qk[k][:, h, :], start=True, stop=True),
                      reads=["qk" + K], writes=["pS" + S])
                ph.op("dve", lambda e, h=h, s=s: e.scalar_tensor_tensor(out=ST[s][:], in0=pS[s][:], scalar=a_s[:, h:h + 1], in1=tri_b[:],
                                                                        op0=ALU.mult, op1=ALU.mult),
                      reads=["pS" + S, "a_s"], writes=["ST" + S])
                ph.op("pe", lambda e, k=k, h=h: e.transpose(pK[:], qk[k][:, 4 + h, :], ident_b[:]), reads=["qk" + K], writes=["pK"])
                ph.op("act", lambda e, h=h, s=s: e.activation(out=kw[s][:], in_=pK[:], func=AF.Copy, scale=wstt[:, h:h + 1]),
                      reads=["pK", "wstt"], writes=["kw" + S])
                ph.op("pe", lambda e, k=k, h=h, s=s: e.matmul(pO[s][:], lhsT=ST[s][:], rhs=vp[k][:, h, :], start=True, stop=False),
                      reads=["ST" + S, "vp" + K], writes=["pO" + S])
                ph.op("pe", lambda e, k=k, h=h, s=s: e.matmul(pO[s][:], lhsT=qk[k][:, h, :], rhs=CTb[h][:], start=False, stop=True),
                      reads=["qk" + K, "CTb%d" % h], writes=["pO" + S])
                ph.op("pe", lambda e, k=k, h=h, s=s: e.matmul(pC[:], lhsT=kw[s][:], rhs=vp[k][:, h, :], start=True, stop=True),
                      reads=["kw" + S, "vp" + K], writes=["pC"])
                ph.op("dve", lambda e, h=h: e.scalar_tensor_tensor(out=CTf[h][:], in0=CTf[h][:], scalar=eg[:, h:h + 1], in1=pC[:],
                                                                   op0=ALU.mult, op1=ALU.add),
                      reads=["CTf%d" % h, "eg", "pC"], writes=["CTf%d" % h])
                ph.op("pool", lambda e, h=h: e.tensor_copy(out=CTb[h][:], in_=CTf[h][:]), reads=["CTf%d" % h], writes=["CTb%d" % h])
                dn, fac, ss1, junk1 = dns[h], facs[h], ss1s[h], junk1s[h]
                DN, FAC, SS1, JK = "dn%d" % h, "fac%d" % h, "ss1%d" % h, "junk1%d" % h
                ph.op("act", lambda e, h=h, s=s, dn=dn, fac=fac, ss1=ss1, junk1=junk1: e.activation(out=dn[:], in_=pO[s][:, 128:129], func=AF.Abs, scale=ebt[:, h:h + 1]),
                      reads=["pO" + S, "ebt"], writes=[DN])
                ph.op("dve", lambda e, dn=dn, fac=fac, ss1=ss1, junk1=junk1: e.tensor_scalar(out=dn[:], in0=dn[:], scalar1=1.0, scalar2=None, op0=ALU.max), reads=[DN], writes=[DN])
                ph.op("dve", lambda e, dn=dn, fac=fac, ss1=ss1, junk1=junk1: e.reciprocal(out=dn[:], in_=dn[:]), reads=[DN], writes=[DN])
                ph.op("dve", lambda e, h=h, dn=dn, fac=fac, ss1=ss1, junk1=junk1: e.tensor_tensor(out=fac[:], in0=dn[:], in1=ebt[:, h:h + 1], op=ALU.mult), reads=[DN, "ebt"], writes=[FAC])
                ph.op("act", lambda e, s=s, dn=dn, fac=fac, ss1=ss1, junk1=junk1: e.activation(out=junk1[:], in_=pO[s][:, 0:128], func=AF.Square, scale=fac[:, 0:1], accum_out=ss1[:]),
                      reads=["pO" + S, FAC], writes=[JK, SS1])
                ph.op("act", lambda e, dn=dn, fac=fac, ss1=ss1, junk1=junk1: e.activation(out=ss1[:], in_=ss1[:], func=AF.Sqrt, bias=epsc[:, 0:1], scale=1.0 / 128), reads=[SS1, "epsc"], writes=[SS1])
                ph.op("dve", lambda e, dn=dn, fac=fac, ss1=ss1, junk1=junk1: e.reciprocal(out=ss1[:], in_=ss1[:]), reads=[SS1], writes=[SS1])
                ph.op("dve", lambda e, dn=dn, fac=fac, ss1=ss1, junk1=junk1: e.tensor_tensor(out=fac[:], in0=fac[:], in1=ss1[:], op=ALU.mult), reads=[FAC, SS1], writes=[FAC])
                ph.op("dve", lambda e, h=h, s=s, y=y, dn=dn, fac=fac, ss1=ss1, junk1=junk1: e.scalar_tensor_tensor(out=ya[y][:, h * 128:(h + 1) * 128], in0=pO[s][:, 0:128], scalar=fac[:, 0:1],
                                                                             in1=GO[:, h * 128:(h + 1) * 128], op0=ALU.mult, op1=ALU.mult),
                      reads=["pO" + S, FAC, "GO"], writes=[("ya", y, h)])
            for c in range(4):
                ph.op("pe", lambda e, c=c, y=y: e.transpose(pY[:, c, :], ya[y][:, c * 128:(c + 1) * 128], ident_b[:]),
                      reads=[("ya", y, c)], writes=["pY"])
            ph.op("act", lambda e, y=y: e.activation(out=yT[y][:], in_=pY[:], func=AF.Copy), reads=["pY"], writes=["yT%d" % y])
            ph.dma("act", yaT[:, tsl].rearrange("(c p) t -> p c t", p=128), yT[y][:], reads=["yT%d" % y], writes=[("yaT", n)])
        ph.emit()

        ph = Phase(nc, f"b2_{li}")
        dqs = [ph.alloc(f"dq{k}", [128, 2, T], BF16) for k in range(2)]
        for k in range(2):
            ph.op("pool", lambda e, k=k: e.memset(dqs[k][:], 0.0), writes=["dq%d" % k])
        dks = [ph.alloc(f"dk{k}", [128, T], BF16) for k in range(2)]
        dvp = [ph.alloc(f"dvp{k}", [128, NT, 129], BF16) for k in range(2)]
        lam4 = ph.alloc("lam4", [128, 256], F32)
        lprod = ph.alloc("lprod", [128, 128], F32)
        lsum = ph.alloc("lsum", [128, 2], F32)
        nlam = ph.alloc("nlam", [128, 1], F32)
        gout = ph.alloc("gout", [128, 128], F32)
        epsc = ph.alloc("epsc", [128, 1], F32)
        ph.op("pool", lambda e: e.memset(epsc[:], EPS), writes=["epsc"])
        bcast_rows(ph, lam4[:], dlam[li], "lam4")
        bcast_rows(ph, gout[:], dout_g[li], "gout")
        ph.op("pool", lambda e: e.tensor_scalar(out=gout[:], in0=gout[:], scalar1=1.0 - lam_init, scalar2=None, op0=ALU.mult),
              reads=["gout"], writes=["gout"])
        l3 = lam4[:].rearrange("p (a d) -> p a d", d=64)
        ph.op("dve", lambda e: e.tensor_tensor(out=lprod[:].rearrange("p (a d) -> p a d", d=64), in0=l3[:, 0:4:2, :], in1=l3[:, 1:4:2, :], op=ALU.mult),
              reads=["lam4"], writes=["lprod"])
        ph.op("dve", lambda e: e.tensor_reduce(out=lsum[:], in_=lprod[:].rearrange("p (a d) -> p a d", d=64), axis=AX.X, op=ALU.add),
              reads=["lprod"], writes=["lsum"])
        ph.op("act", lambda e: e.activation(out=lsum[:], in_=lsum[:], func=AF.Exp), reads=["lsum"], writes=["lsum"])
        ph.op("dve", lambda e: e.tensor_tensor(out=nlam[:], in0=lsum[:, 1:2], in1=lsum[:, 0:1], op=ALU.subtract), reads=["lsum"], writes=["nlam"])
        ph.op("dve", lambda e: e.tensor_scalar(out=nlam[:], in0=nlam[:], scalar1=-lam_init, scalar2=None, op0=ALU.add), reads=["nlam"], writes=["nlam"])
        pS2 = [ph.psum(f"pS{k}", [128, 512], F32) for k in range(2)]
        pO2 = [[ph.psum(f"pO{c}{k}", [128, 2, 129], F32) for k in range(2)] for c in range(2)]
        pY2 = ph.psum("pY", [128, 128], BF16)
        PT = [ph.alloc(f"PT{k}", [128, 512], BF16) for k in range(2)]
        r01 = ph.alloc("r01", [128, 2], F32)
        t1_ = ph.alloc("t1_", [128, 128], F32)
        o_ = ph.alloc("o_", [128, 128], F32)
        ss2 = ph.alloc("ss2", [128, 1], F32)
        junk2 = ph.alloc("junk2", [128, 128], BF16)
        yc = [ph.alloc(f"yc{k}", [128, 128], BF16) for k in range(2)]
        ycTt = [ph.alloc(f"ycT{k}", [128, 128], BF16) for k in range(2)]
        for k in range(2):
            ph.op("pool", lambda e, k=k: e.memset(dvp[k][:], 1.0), writes=["dvp%d" % k])
        cntS = 0
        cntY = 0
        NQB = T // 256
        for h in range(4):
            hb = h % 2
            HB = str(hb)
            ph.dma("sp", dqs[hb][0:64, 0, :], dqT[h * 128:h * 128 + 64, :], reads=["dq" + HB], writes=["dqa" + HB])
            ph.dma("sp", dqs[hb][64:128, 1, :], dqT[h * 128 + 64:(h + 1) * 128, :], reads=["dq" + HB], writes=["dqb" + HB])
            ph.dma("sp", dks[hb][:], dkT[h * 128:(h + 1) * 128, :], writes=["dk" + HB])
            ph.dma("sp", dvp[hb][:, :, 0:128], dv[:, h * 128:(h + 1) * 128].rearrange("(n p) v -> p n v", p=128), writes=["dvp" + HB])
            for qb in range(NQB):
                ob_ = qb % 2
                qsl = slice(qb * 256, (qb + 1) * 256)
                first = [True, True]
                for j in range(2 * qb + 2):
                    s = cntS % 2
                    cntS += 1
                    S = str(s)
                    jsl = slice(j * 128, (j + 1) * 128)
                    for c in range(2):
                        ph.op("pe", lambda e, c=c, s=s, hb=hb, jsl=jsl, qsl=qsl: e.matmul(
                            pS2[s][:, c * 256:(c + 1) * 256], lhsT=dks[hb][:, jsl], rhs=dqs[hb][:, c, qsl],
                            start=(c == 0), stop=False, skip_group_check=True),
                            reads=["dqa" + HB, "dqb" + HB, "dk" + HB], writes=["pS" + S])
                    if j >= 2 * qb:
                        sub = j - 2 * qb
                        for c in range(2):
                            ph.op("pe", lambda e, c=c, s=s, sub=sub: e.matmul(
                                pS2[s][:, c * 256 + sub * 128:c * 256 + (sub + 1) * 128], lhsT=ident_b[:], rhs=mb_b[:],
                                start=False, stop=True, skip_group_check=True), reads=[], writes=["pS" + S])
                    ph.op("act", lambda e, s=s: e.activation(out=PT[s][:], in_=pS2[s][:], func=AF.Exp), reads=["pS" + S], writes=["PT" + S])
                    subs = (0, 1) if j <= 2 * qb else (1,)
                    for sub in subs:
                        for c in range(2):
                            st = first[c]
                            first[c] = False
                            ph.op("pe", lambda e, c=c, s=s, sub=sub, st=st, hb=hb, j=j, ob_=ob_: e.matmul(
                                pO2[c][ob_][:, sub, :], lhsT=PT[s][:, c * 256 + sub * 128:c * 256 + (sub + 1) * 128], rhs=dvp[hb][:, j, :],
                                start=st, stop=False, skip_group_check=True),
                                reads=["PT" + S, "dvp" + HB], writes=[("pO", c, ob_)])
                for sub in range(2):
                    i = 2 * qb + sub
                    tsl = slice(i * 128, (i + 1) * 128)
                    yk = cntY % 2
                    cntY += 1
                    ph.op("dve", lambda e, ob_=ob_, sub=sub: e.reciprocal(out=r01[:, 0:1], in_=pO2[0][ob_][:, sub, 128:129]),
                          reads=[("pO", 0, ob_)], writes=["r0"])
                    ph.op("dve", lambda e, ob_=ob_, sub=sub: e.reciprocal(out=r01[:, 1:2], in_=pO2[1][ob_][:, sub, 128:129]),
                          reads=[("pO", 1, ob_)], writes=["r1"])
                    ph.op("dve", lambda e: e.tensor_tensor(out=r01[:, 1:2], in0=r01[:, 1:2], in1=nlam[:], op=ALU.mult), reads=["r1", "nlam"], writes=["r1"])
                    ph.op("dve", lambda e, ob_=ob_, sub=sub: e.tensor_scalar(out=t1_[:], in0=pO2[1][ob_][:, sub, 0:128], scalar1=r01[:, 1:2], scalar2=None,
                                                                             op0=ALU.mult), reads=[("pO", 1, ob_), "r1"], writes=["t1_"])
                    ph.op("dve", lambda e, ob_=ob_, sub=sub: e.scalar_tensor_tensor(out=o_[:], in0=pO2[0][ob_][:, sub, 0:128], scalar=r01[:, 0:1], in1=t1_[:],
                                                                                    op0=ALU.mult, op1=ALU.add),
                          reads=[("pO", 0, ob_), "r0", "t1_"], writes=["o_"])
                    ph.op("act", lambda e: e.activation(out=junk2[:], in_=o_[:], func=AF.Square, accum_out=ss2[:]), reads=["o_"], writes=["junk2", "ss2"])
                    ph.op("act", lambda e: e.activation(out=ss2[:], in_=ss2[:], func=AF.Sqrt, bias=epsc[:, 0:1], scale=1.0 / 128), reads=["ss2", "epsc"], writes=["ss2"])
                    ph.op("dve", lambda e: e.reciprocal(out=ss2[:], in_=ss2[:]), reads=["ss2"], writes=["ss2"])
                    ph.op("dve", lambda e, yk=yk: e.scalar_tensor_tensor(out=yc[yk][:], in0=o_[:], scalar=ss2[:, 0:1], in1=gout[:], op0=ALU.mult, op1=ALU.mult),
                          reads=["o_", "ss2", "gout"], writes=["yc%d" % yk])
                    ph.op("pe", lambda e, yk=yk: e.transpose(pY2[:], yc[yk][:], ident_b[:]), reads=["yc%d" % yk], writes=["pY"])
                    ph.op("act", lambda e, yk=yk: e.activation(out=ycTt[yk][:], in_=pY2[:], func=AF.Copy), reads=["pY"], writes=["ycT%d" % yk])
                    ph.dma("act", ycT[h * 128:(h + 1) * 128, tsl], ycTt[yk][:], reads=["ycT%d" % yk], writes=[("ycT", h, i)])
        ph.emit()

        ph = Phase(nc, f"b3_{li}")
        ik2 = ph.alloc("ik2", [128, T], F32)
        skp = ph.alloc("skp", [128, 4, T], BF16)
        svp = ph.alloc("svp", [128, NT, 8, 65], BF16)
        iqb = ph.alloc("iqb", [128, 4, 512], F32)
        sqb = ph.alloc("sqb", [128, 8, 512], BF16)
        ph.op("pool", lambda e: e.memset(iqb[:], 0.0), writes=["iqb"])
        ph.op("pool", lambda e: e.memset(sqb[:], 0.0), writes=["sqb"])
        iwb = ph.alloc("iwb", [128, 4, 4], F32)
        accs = [ph.alloc(f"acc{k}", [128, T], F32) for k in range(2)]
        MBT = ph.alloc("MBT", [128, NT, 512], BF16)
        mq01 = ph.alloc("mq01", [128, T], BF16)
        rr_ = [ph.alloc(f"rr{k}", [128, 512], F32) for k in range(2)]
        PT3 = [ph.alloc(f"PT{k}", [128, 512], BF16) for k in range(2)]
        hi = ph.alloc("hi", [128, 1], F32)
        lo = ph.alloc("lo", [128, 1], F32)
        steps = ph.alloc("steps", [128, NBIS + 1], F32)
        pw2 = ph.alloc("pw2", [128, NBIS + 1], F32)
        thr = ph.alloc("thr", [128, 1], F32)
        cntt = ph.alloc("cntt", [128, 1], F32)
        gg = ph.alloc("gg", [128, 1], F32)
        rec = ph.alloc("rec", [128, 4], F32)
        yb = ph.alloc("yb", [128, 4, 512], BF16)
        ybTt = [ph.alloc(f"ybT{k}", [128, 4, 128], BF16) for k in range(2)]
        pI3 = [ph.psum(f"pI{k}", [128, 512], F32) for k in range(2)]
        pM = ph.psum("pM", [128, 4, 128], BF16)
        pS3 = [ph.psum(f"pS{k}", [128, 512], F32) for k in range(2)]
        pO3 = [ph.psum(f"pO{k}", [128, 4, 65], F32) for k in range(2)]
        pY3 = ph.psum("pY", [128, 4, 128], BF16)
        for j in range(NBIS + 1):
            ph.op("pool", lambda e, j=j: e.memset(pw2[:, j:j + 1], 0.5 ** (j + 1)), writes=["pw2"])
        ph.op("pool", lambda e: e.memset(svp[:], 1.0), writes=["svp"])
        ph.op("pool", lambda e: e.memset(MBT[:], -30000.0), writes=["MBT"])
        ph.dma("sp", ik2[0:64, :], ikT, writes=["ik2a"])
        ph.dma("sp", ik2[64:128, :], ikT, writes=["ik2b"])
        ph.dma("sp", skp[:], skT.rearrange("(g p) t -> p g t", p=128), writes=["skp"])
        for n in range(NT):
            ph.dma("sp", svp[:, n, :, 0:64], sv[n * 128:(n + 1) * 128, :].rearrange("t (h v) -> t h v", v=64), reads=["svp"], writes=[("svp", n)])
        NQ4 = T // 512
        cntI = 0
        cntS = 0
        cntO = 0
        cntY = 0
        for QB in range(NQ4):
            qbs = slice(QB * 512, (QB + 1) * 512)
            iq4 = iqT.rearrange("(g two d) t -> two d g t", two=2, d=64)
            sq4 = sqT.rearrange("(g two d) t -> two d g t", two=2, d=64)
            ph.dma("sp", iqb[0:64, 0:4:2, :], iq4[0][:, :, qbs], reads=["iqb"], writes=["iqba"])
            ph.dma("sp", iqb[64:128, 1:4:2, :], iq4[1][:, :, qbs], reads=["iqb"], writes=["iqbb"])
            ph.dma("sp", sqb[0:64, 0:8:2, :], sq4[0][:, :, qbs], reads=["sqb"], writes=["sqba"])
            ph.dma("sp", sqb[64:128, 1:8:2, :], sq4[1][:, :, qbs], reads=["sqb"], writes=["sqbb"])
            ph.dma("sp", iwb[:], iw[qbs, :].rearrange("(s p) h -> p s h", p=128), writes=["iwb"])
            for sub in range(4):
                i = QB * 4 + sub
                a = i % 2
                A = "acc%d" % a
                acc_ = accs[a]
                Tk = (i + 1) * 128
                nkb = (Tk + 511) // 512
                for kb in range(nkb):
                    w = min(512, Tk - kb * 512)
                    ksl = slice(kb * 512, kb * 512 + w)
                    for hh in range(4):
                        p = cntI % 2
                        cntI += 1
                        P = "pI%d" % p
                        half = hh % 2
                        hs = slice(half * 64, (half + 1) * 64)
                        ph.op("pe", lambda e, p=p, w=w, hs=hs, hh=hh, sub=sub, ksl=ksl: e.matmul(
                            pI3[p][:, 0:w], lhsT=iqb[:, hh, sub * 128:(sub + 1) * 128], rhs=ik2[:, ksl], start=True, stop=True),
                            reads=["iqba", "iqbb", "ik2a", "ik2b"], writes=[P])
                        ph.op("act", lambda e, p=p, w=w: e.activation(out=rr_[p][:, 0:w], in_=pI3[p][:, 0:w], func=AF.Relu), reads=[P], writes=["rr%d" % p])
                        if hh == 0:
                            ph.op("dve", lambda e, p=p, w=w, ksl=ksl, sub=sub, acc_=acc_: e.tensor_scalar(
                                out=acc_[:, ksl], in0=rr_[p][:, 0:w], scalar1=iwb[:, sub, 0:1], scalar2=None, op0=ALU.mult),
                                reads=["rr%d" % p, "iwb"], writes=[A])
                        else:
                            ph.op("dve", lambda e, p=p, w=w, ksl=ksl, sub=sub, hh=hh, acc_=acc_: e.scalar_tensor_tensor(
                                out=acc_[:, ksl], in0=rr_[p][:, 0:w], scalar=iwb[:, sub, hh:hh + 1], in1=acc_[:, ksl], op0=ALU.mult, op1=ALU.add),
                                reads=["rr%d" % p, "iwb", A], writes=[A])
                if Tk > TOPK:
                    ph.op("dve", lambda e, acc_=acc_, Tk=Tk: e.tensor_reduce(out=hi[:], in_=acc_[:, 0:Tk], axis=AX.X, op=ALU.max), reads=[A], writes=["hi"])
                    ph.op("dve", lambda e, acc_=acc_, Tk=Tk: e.tensor_reduce(out=lo[:], in_=acc_[:, 0:Tk], axis=AX.X, op=ALU.min), reads=[A], writes=["lo"])
                ph.op("dve", lambda e, acc_=acc_, Tk=Tk: e.tensor_tensor(out=acc_[:, Tk - 128:Tk], in0=acc_[:, Tk - 128:Tk], in1=mbq_f[:], op=ALU.add),
                      reads=[A], writes=[A])
                if Tk > TOPK:
                    ph.op("dve", lambda e: e.tensor_tensor(out=hi[:], in0=hi[:], in1=lo[:], op=ALU.subtract), reads=["hi", "lo"], writes=["hi"])
                    ph.op("dve", lambda e: e.tensor_scalar(out=steps[:], in0=pw2[:], scalar1=hi[:, 0:1], scalar2=None, op0=ALU.mult),
                          reads=["pw2", "hi"], writes=["steps"])
                    ph.op("dve", lambda e: e.tensor_tensor(out=thr[:], in0=lo[:], in1=steps[:, 0:1], op=ALU.add), reads=["lo", "steps"], writes=["thr"])
                    for it in range(NBIS):
                        ph.op("dve", lambda e, acc_=acc_, Tk=Tk: e.tensor_scalar(out=mq01[:, 0:Tk], in0=acc_[:, 0:Tk], scalar1=thr[:, 0:1], scalar2=None,
                                                                                 op0=ALU.is_ge, op1=ALU.add, accum_out=cntt[:]),
                              reads=[A, "thr"], writes=["mq01", "cntt"])
                        ph.op("dve", lambda e, it=it: e.scalar_tensor_tensor(out=gg[:], in0=cntt[:], scalar=float(TOPK), in1=steps[:, it:it + 1],
                                                                             op0=ALU.is_ge, op1=ALU.mult), reads=["cntt", "steps"], writes=["gg"])
                        ph.op("dve", lambda e, it=it: e.scalar_tensor_tensor(out=thr[:], in0=gg[:], scalar=steps[:, it + 1:it + 2], in1=thr[:],
                                                                             op0=ALU.subtract, op1=ALU.add), reads=["gg", "steps", "thr"], writes=["thr"])
                    ph.op("dve", lambda e: e.scalar_tensor_tensor(out=thr[:], in0=steps[:, NBIS:NBIS + 1], scalar=-3.0, in1=thr[:],
                                                                  op0=ALU.mult, op1=ALU.add), reads=["thr", "steps"], writes=["thr"])
                else:
                    ph.op("dve", lambda e: e.memset(thr[:], -1e29), writes=["thr"])
                ph.op("dve", lambda e, acc_=acc_, Tk=Tk: e.tensor_scalar(out=mq01[:, 0:Tk], in0=acc_[:, 0:Tk], scalar1=thr[:, 0:1], scalar2=one_c[:, 0:1],
                                                                         op0=ALU.is_ge, op1=ALU.subtract), reads=[A, "thr"], writes=["mq01"])
                for j0 in range(0, i + 1, 4):
                    nj = min(4, i + 1 - j0)
                    for jj in range(nj):
                        ph.op("pe", lambda e, jj=jj, j0=j0: e.transpose(pM[:, jj, :], mq01[:, (j0 + jj) * 128:(j0 + jj + 1) * 128], ident_b[:]),
                              reads=["mq01"], writes=["pM"])
                    ph.op("act", lambda e, nj=nj, j0=j0, sub=sub: e.activation(out=MBT[:, j0:j0 + nj, sub * 128:(sub + 1) * 128], in_=pM[:, 0:nj, :],
                                                                               func=AF.Copy, scale=30000.0), reads=["pM"], writes=["MBT"])
            njt = QB * 4 + 4
            for h in range(8):
                o = cntO % 2
                cntO += 1
                O = "pO%d" % o
                half = h % 2
                hs = slice(half * 64, (half + 1) * 64)
                firstO = True
                for j in range(njt):
                    s = cntS % 2
                    cntS += 1
                    S = str(s)
                    ph.op("pe", lambda e, s=s, j=j: e.matmul(pS3[s][:], lhsT=ident_b[:], rhs=MBT[:, j, :], start=True, stop=False, skip_group_check=True),
                          reads=["MBT"], writes=["pS" + S])
                    ph.op("pe", lambda e, s=s, j=j, hs=hs, h=h: e.matmul(pS3[s][:], lhsT=skp[:, h // 2, j * 128:(j + 1) * 128], rhs=sqb[:, h, :],
                                                                         start=False, stop=True, skip_group_check=True),
                          reads=["skp", "sqba", "sqbb"], writes=["pS" + S])
                    ph.op("act", lambda e, s=s: e.activation(out=PT3[s][:], in_=pS3[s][:], func=AF.Exp), reads=["pS" + S], writes=["PT" + S])
                    for sub in range(4):
                        if QB * 4 + sub < j:
                            continue
                        st = firstO
                        firstO = False
                        ph.op("pe", lambda e, s=s, sub=sub, o=o, j=j, h=h, st=st: e.matmul(
                            pO3[o][:, sub, :], lhsT=PT3[s][:, sub * 128:(sub + 1) * 128], rhs=svp[:, j, h, :], start=st, stop=False, skip_group_check=True),
                            reads=["PT" + S, ("svp", j)], writes=[O])
                ph.op("dve", lambda e, o=o: e.reciprocal(out=rec[:], in_=pO3[o][:, :, 64]), reads=[O], writes=["rec"])
                ph.op("dve", lambda e, o=o, h=h: e.tensor_tensor(out=yb[:, :, h * 64:(h + 1) * 64], in0=pO3[o][:, :, 0:64],
                                                                 in1=rec[:].unsqueeze(2).to_broadcast([128, 4, 64]), op=ALU.mult),
                      reads=[O, "rec"], writes=["yb"])
            for sub in range(4):
                i = QB * 4 + sub
                yk = cntY % 2
                cntY += 1
                for c in range(4):
                    ph.op("pe", lambda e, c=c, sub=sub: e.transpose(pY3[:, c, :], yb[:, sub, c * 128:(c + 1) * 128], ident_b[:]), reads=["yb"], writes=["pY"])
                ph.op("act", lambda e, yk=yk: e.activation(out=ybTt[yk][:], in_=pY3[:], func=AF.Copy), reads=["pY"], writes=["ybT%d" % yk])
                ph.dma("act", ybT[:, i * 128:(i + 1) * 128].rearrange("(c p) t -> p c t", p=128), ybTt[yk][:], reads=["ybT%d" % yk], writes=[("ybT", i)])
        ph.emit()

        ph = Phase(nc, f"c1_{li}")
        wbr = ph.alloc("wbr", [128, 12, D], BF16)
        wo = ph.alloc("wo", [128, 8, D], BF16)
        wstg = [ph.alloc(f"wstg{k}", [128, 2, D], F32) for k in range(2)]
        for g2_ in range(10):
            g = g2_ // 2
            hf2 = g2_ % 2
            k = g2_ % 2
            K = str(k)
            if g < 3:
                src = w_branch[li, g][hf2 * 256:(hf2 + 1) * 256, :].rearrange("(c p) n -> p c n", p=128)
                dstw = wbr[:, g * 4 + hf2 * 2:g * 4 + hf2 * 2 + 2, :]
            else:
                src = w_out[li][(g - 3) * 512 + hf2 * 256:(g - 3) * 512 + (hf2 + 1) * 256, :].rearrange("(c p) n -> p c n", p=128)
                dstw = wo[:, (g - 3) * 4 + hf2 * 2:(g - 3) * 4 + hf2 * 2 + 2, :]
            ph.dma("sp", wstg[k][:], src, writes=["wstg" + K])
            ph.op("dve" if k == 0 else "pool", lambda e, k=k, dstw=dstw: e.tensor_copy(out=dstw, in_=wstg[k][:]),
                  reads=["wstg" + K] + ([("w", g)] if hf2 else []), writes=[("w", g)])
        yts_ = [ph.alloc(f"yt{b}", [128, 4, 512], BF16) for b in range(3)]
        yts = [[yts_[b], yts_[b]] for b in range(3)]
        gts_ = [ph.alloc(f"gt{b}", [128, 8, 512], BF16) for b in range(3)]
        gts = [[gts_[b], gts_[b]] for b in range(3)]
        mrg = [ph.alloc(f"mrg{k}", [128, 8, 512], BF16) for k in range(2)]
        m1 = [ph.alloc(f"m1{k}", [128, 512], F32) for k in range(3)]
        m2 = ph.alloc("m2s", [128, 512], F32)
        xt2 = [ph.alloc(f"xt{k}", [128, D], F32) for k in range(2)]
        x1t = [ph.alloc(f"x1t{k}", [128, D], F32) for k in range(2)]
        xh2 = [ph.alloc(f"xh{k}", [128, D], BF16) for k in range(2)]
        xT2 = [ph.alloc(f"xT{k}", [128, 8, 128], BF16) for k in range(2)]
        junk4 = ph.alloc("junk4", [128, D], BF16)
        ss4 = ph.alloc("ss4", [128, 1], F32)
        epsc = ph.alloc("epsc", [128, 1], F32)
        ph.op("pool", lambda e: e.memset(epsc[:], EPS), writes=["epsc"])
        pMg = [ph.psum(f"pM{b}", [128, 512], F32) for b in range(3)]
        pX = [ph.psum(f"pX{k}", [128, 512], F32) for k in range(2)]
        pT4 = ph.psum("pT4", [128, 8, 128], BF16)
        ysrc = [yaT, ybT, ycT]
        for tb in range(T // 512):
            k = tb % 2
            K = str(k)
            bsl = slice(tb * 512, (tb + 1) * 512)
            for b in range(3):
                ph.dma("sp", yts[b][k][:], ysrc[b][:, bsl].rearrange("(c p) t -> p c t", p=128), writes=["yt%d" % b])
                ph.dma("sp", gts[b][k][:], gates[b * D:(b + 1) * D, bsl].rearrange("(c p) t -> p c t", p=128), writes=["gt%d" % b])
            for dc in range(8):
                for b in range(3):
                    for c in range(4):
                        ph.op("pe", lambda e, b=b, c=c, dc=dc, k=k: e.matmul(pMg[b][:], lhsT=wbr[:, b * 4 + c, dc * 128:(dc + 1) * 128], rhs=yts[b][k][:, c, :],
                                                                             start=(c == 0), stop=(c == 3)),
                              reads=[("w", b), "yt%d" % b], writes=["pM%d" % b])
                    ph.op("dve", lambda e, b=b, dc=dc, k=k: e.tensor_tensor(out=m1[b][:], in0=pMg[b][:], in1=gts[b][k][:, dc, :], op=ALU.mult),
                          reads=["pM%d" % b, "gt%d" % b], writes=["m1%d" % b])
                ph.op("dve", lambda e: e.tensor_tensor(out=m2[:], in0=m1[0][:], in1=m1[1][:], op=ALU.add), reads=["m10", "m11"], writes=["m2"])
                ph.op("dve", lambda e, dc=dc, k=k: e.tensor_tensor(out=mrg[k][:, dc, :], in0=m2[:], in1=m1[2][:], op=ALU.add),
                      reads=["m2", "m12"], writes=["mrg" + K])
            for tt in range(4):
                i = tb * 4 + tt
                x = i % 2
                X = str(x)
                tsl = slice(i * 128, (i + 1) * 128)
                ph.dma("sp", xt2[x][:], xsrc[tsl, :], writes=["xt" + X])
                for dh in range(2):
                    p = (i * 2 + dh) % 2
                    for c in range(8):
                        ph.op("pe", lambda e, p=p, c=c, k=k, tt=tt, dh=dh: e.matmul(pX[p][:], lhsT=mrg[k][:, c, tt * 128:(tt + 1) * 128],
                                                                                    rhs=wo[:, c, dh * 512:(dh + 1) * 512], start=(c == 0), stop=(c == 7)),
                              reads=["mrg" + K, ("w", 3), ("w", 4)], writes=["pX%d" % p])
                    ph.op("dve", lambda e, p=p, x=x, dh=dh: e.tensor_tensor(out=x1t[x][:, dh * 512:(dh + 1) * 512], in0=pX[p][:],
                                                                            in1=xt2[x][:, dh * 512:(dh + 1) * 512], op=ALU.add),
                          reads=["pX%d" % p, "xt" + X], writes=["x1t" + X])
                ph.dma("pool", x1[tsl, :], x1t[x][:], reads=["x1t" + X], writes=[("x1", i)])
                ph.op("act", lambda e, x=x: e.activation(out=junk4[:], in_=x1t[x][:], func=AF.Square, accum_out=ss4[:]), reads=["x1t" + X], writes=["junk4", "ss4"])
                ph.op("act", lambda e: e.activation(out=ss4[:], in_=ss4[:], func=AF.Sqrt, bias=epsc[:, 0:1], scale=1.0 / D), reads=["ss4", "epsc"], writes=["ss4"])
                ph.op("dve", lambda e: e.reciprocal(out=ss4[:], in_=ss4[:]), reads=["ss4"], writes=["ss4"])
                ph.op("dve", lambda e, x=x: e.tensor_scalar(out=xh2[x][:], in0=x1t[x][:], scalar1=ss4[:, 0:1], scalar2=None, op0=ALU.mult),
                      reads=["x1t" + X, "ss4"], writes=["xh" + X])
                for c in range(8):
                    ph.op("pe", lambda e, c=c, x=x: e.transpose(pT4[:, c, :], xh2[x][:, c * 128:(c + 1) * 128], ident_b[:]), reads=["xh" + X], writes=["pT4"])
                ph.op("act", lambda e, x=x: e.activation(out=xT2[x][:], in_=pT4[:], func=AF.Copy), reads=["pT4"], writes=["xT" + X])
                ph.dma("act", xn2T[:, tsl].rearrange("(c p) t -> p c t", p=128), xT2[x][:], reads=["xT" + X], writes=[("xn2T", i)])
        ph.emit()

        ph = Phase(nc, f"c2_{li}")
        NF = FF // 128
        wd = ph.alloc("wd", [128, NF, D], BF16)
        wds = [ph.alloc(f"wds{k}", [128, 2, D], F32) for k in range(2)]
        g2 = ph.alloc("g2", [128, 8], F32)
        load_vec_cols(ph, g2[:], norm_ffn_g[li], "g2")
        for g in range(NF // 2):
            k = g % 2
            ph.dma("sp", wds[k][:], w_down[li][g * 256:(g + 1) * 256, :].rearrange("(c p) n -> p c n", p=128), writes=["wds%d" % k])
            ph.op("dve" if g % 2 == 0 else "pool", lambda e, k=k, g=g: e.tensor_copy(out=wd[:, 2 * g:2 * g + 2, :], in_=wds[k][:]),
                  reads=["wds%d" % k], writes=[("wd", g)])
        TBF = min(1024, T)
        xb = [ph.alloc(f"xb{k}", [128, 8, TBF], BF16) for k in range(2)]
        hT = ph.alloc("hT", [128, NF, TBF], BF16)
        wgs = [ph.alloc(f"wgs{k}", [128, 8, 256], F32) for k in range(2)]
        wg = [ph.alloc(f"wg{k}", [128, 8, 256], BF16) for k in range(2)]
        sg = [ph.alloc(f"sg{k}", [128, 512], F32) for k in range(2)]
        x1b = [ph.alloc(f"x1b{k}", [128, D], F32) for k in range(2)]
        ot = [ph.alloc(f"ot{k}", [128, D], F32) for k in range(2)]
        pG = [ph.psum(f"pG{k}", [128, 512], F32) for k in range(2)]
        pU = [ph.psum(f"pU{k}", [128, 512], F32) for k in range(2)]
        pD = [ph.psum(f"pD{k}", [128, 512], F32) for k in range(2)]
        cntW = 0
        cntG = 0
        for blk in range(T // TBF):
            k = blk % 2
            K = str(k)
            bsl = slice(blk * TBF, (blk + 1) * TBF)
            ph.dma("sp", xb[k][:], xn2T[:, bsl].rearrange("(c p) t -> p c t", p=128), writes=["xb" + K])
            for f in range(NF):
                w = cntW % 2
                cntW += 1
                W = str(w)
                ph.dma("sp", wgs[w][:, :, 0:128], w_gate_up[li][:, f * 128:(f + 1) * 128].rearrange("(c p) n -> p c n", p=128), writes=["wgsa" + W])
                ph.dma("sp", wgs[w][:, :, 128:256], w_gate_up[li][:, FF + f * 128:FF + (f + 1) * 128].rearrange("(c p) n -> p c n", p=128), writes=["wgsb" + W])
                ph.op("pool", lambda e, w=w: e.tensor_tensor(out=wg[w][:], in0=wgs[w][:], in1=g2[:].unsqueeze(2).to_broadcast([128, 8, 256]), op=ALU.mult),
                      reads=["wgsa" + W, "wgsb" + W, "g2"], writes=["wg" + W])
                for hf in range(TBF // 512):
                    q = cntG % 2
                    cntG += 1
                    Q = str(q)
                    hsl = slice(hf * 512, (hf + 1) * 512)
                    for c in range(8):
                        ph.op("pe", lambda e, w=w, q=q, c=c, k=k, hsl=hsl: e.matmul(pG[q][:], lhsT=wg[w][:, c, 0:128], rhs=xb[k][:, c, hsl], start=(c == 0), stop=(c == 7)),
                              reads=["wg" + W, "xb" + K], writes=["pG" + Q])
                    for c in range(8):
                        ph.op("pe", lambda e, w=w, q=q, c=c, k=k, hsl=hsl: e.matmul(pU[q][:], lhsT=wg[w][:, c, 128:256], rhs=xb[k][:, c, hsl], start=(c == 0), stop=(c == 7)),
                              reads=["wg" + W, "xb" + K], writes=["pU" + Q])
                    ph.op("act", lambda e, q=q: e.activation(out=sg[q][:], in_=pG[q][:], func=AF.Silu), reads=["pG" + Q], writes=["sg" + Q])
                    ph.op("dve", lambda e, q=q, f=f, hsl=hsl: e.tensor_tensor(out=hT[:, f, hsl], in0=sg[q][:], in1=pU[q][:], op=ALU.mult),
                          reads=["sg" + Q, "pU" + Q], writes=[("hT", f)])
            for tt in range(TBF // 128):
                i = blk * (TBF // 128) + tt
                x = i % 2
                X = str(x)
                tsl = slice(i * 128, (i + 1) * 128)
                ph.dma("sp", x1b[x][:], x1[tsl, :], writes=["x1b" + X])
                for dh in range(2):
                    p = (i * 2 + dh) % 2
                    for f in range(NF):
                        ph.op("pe", lambda e, p=p, f=f, tt=tt, dh=dh: e.matmul(pD[p][:], lhsT=hT[:, f, tt * 128:(tt + 1) * 128], rhs=wd[:, f, dh * 512:(dh + 1) * 512],
                                                                               start=(f == 0), stop=(f == NF - 1)),
                              reads=[("hT", f), ("wd", f // 2)], writes=["pD%d" % p])
                    ph.op("dve", lambda e, p=p, x=x, dh=dh: e.tensor_tensor(out=ot[x][:, dh * 512:(dh + 1) * 512], in0=pD[p][:],
                                                                            in1=x1b[x][:, dh * 512:(dh + 1) * 512], op=ALU.add),
                          reads=["pD%d" % p, "x1b" + X], writes=["ot" + X])
                ph.dma("pool", xdst[tsl, :], ot[x][:], reads=["ot" + X], writes=[("xdst", i)])
        ph.emit()
    top.close()
    return nc


_CACHE = {}


def _rope_tab(T):
    inv = 1.0 / np.power(np.float32(10000.0), np.arange(0, 64, 2, dtype=np.float32) / np.float32(64))
    ang = np.arange(T, dtype=np.float32)[:, None] * inv[None, :].astype(np.float32)
    return np.concatenate([np.cos(ang), np.sin(ang)], axis=1).astype(np.float32)


def kernel(**inputs):
    x = np.asarray(inputs["x"], dtype=np.float32)
    B, T, _ = x.shape
    L = inputs["w_in"].shape[0]
    key = (T, L)
    if key not in _CACHE:
        _CACHE[key] = build(T, L)
    nc = _CACHE[key]
    shared = {}
    for k, v in inputs.items():
        if k == "x":
            continue
        a = np.ascontiguousarray(np.asarray(v, dtype=np.float32))
        if k in ("mlstm_norm_g", "diff_lambda"):
            a = a.reshape(L, -1)
        shared[k] = a
    shared["rope_cs"] = _rope_tab(T)
    in_maps = [dict(shared, x=np.ascontiguousarray(x[b])) for b in range(B)]
    res = run_bass_kernel_spmd(nc, in_maps, core_ids=list(range(B)))
    return np.stack([np.asarray(r["out"]) for r in res.results], axis=0).astype(np.float32)
```

```python
import math
from contextlib import ExitStack

import numpy as np
import concourse.bass as bass
import concourse.mybir as mybir
from concourse.bass_utils import run_bass_kernel_spmd

F32 = mybir.dt.float32
BF16 = mybir.dt.bfloat16
AF = mybir.ActivationFunctionType
ALU = mybir.AluOpType
AX = mybir.AxisListType

D = 1024
FF = 2816
IN_COLS = 7756
EPS = 1e-6
ENGS = ("pe", "act", "dve", "pool", "sp")


class Op:
    __slots__ = ("eng", "fn", "deps", "signal", "sigval", "chan", "idx", "isdma")

    def __init__(self, eng, fn, chan=None):
        self.eng = eng
        self.fn = fn
        self.deps = set()
        self.signal = False
        self.sigval = 0
        self.chan = chan
        self.isdma = chan is not None
        self.idx = -1


def _is_psum_key(k):
    if isinstance(k, tuple):
        return k[0] == "pO"
    return isinstance(k, str) and len(k) > 1 and k[0] == "p" and k != "pw2"


class Phase:
    def __init__(self, nc, name, nchan=12):
        self.nc = nc
        self.name = name
        self.ops = []
        self.last_w = {}
        self.readers = {}
        self.chan_last = {}
        self.nchan = nchan
        self.rr = 0
        self.es = ExitStack()

    def alloc(self, name, shape, dt):
        return self.es.enter_context(self.nc.sbuf_tensor(f"{self.name}_{name}", list(shape), dt))

    def psum(self, name, shape, dt=F32):
        shape = list(shape)
        n = 1
        for s_ in shape[1:]:
            n *= s_
        per_bank = 512 if dt == F32 else 1024
        nb = (n + per_bank - 1) // per_bank
        t = self.es.enter_context(self.nc.psum_tensor(f"{self.name}_{name}", [128, nb * per_bank], dt))
        v = t[:, 0:n]
        if len(shape) == 3:
            v = v.rearrange("p (a b) -> p a b", b=shape[2])
        return v

    oplimit = 10 ** 9

    def _add(self, op, reads, writes):
        if len(self.ops) >= Phase.oplimit and Phase.count + 1 == Phase.limit:
            return op
        op.idx = len(self.ops)
        deps = set()
        for r in reads:
            w = self.last_w.get(r)
            if w is not None:
                deps.add(w)
            if _is_psum_key(r):
                for rd in self.readers.get(r, ()):
                    if self.ops[rd].eng != op.eng:
                        deps.add(rd)
        for r in writes:
            w = self.last_w.get(r)
            if w is not None:
                deps.add(w)
            for rd in self.readers.get(r, ()):
                deps.add(rd)
        if op.isdma:
            prev = self.chan_last.get(op.chan)
            if prev is not None:
                deps.add(prev)
            self.chan_last[op.chan] = op.idx
        deps.discard(op.idx)
        op.deps = deps
        self.ops.append(op)
        for r in writes:
            self.last_w[r] = op.idx
            self.readers[r] = []
        for r in reads:
            if r not in writes:
                self.readers.setdefault(r, []).append(op.idx)
        return op

    def op(self, eng, fn, reads=(), writes=()):
        return self._add(Op(eng, fn), tuple(reads), tuple(writes))

    def dma(self, q, out, in_, reads=(), writes=(), slow=False):
        chan = self.rr % self.nchan
        self.rr += 1
        if slow:
            fn = lambda e: e.dma_start(out=out, in_=in_, allow_slow_non_contiguous=True)
        else:
            fn = lambda e: e.dma_start(out=out, in_=in_)
        return self._add(Op(q, fn, chan=chan), tuple(reads), tuple(writes))

    count = 0
    limit = 10 ** 9
    verbose = False
    trace = None
    gsem = None

    def emit(self):
        Phase.count += 1
        if Phase.verbose:
            print("phase", self.name, "ops", len(self.ops), "sbuf_remaining", self.nc.sbuf_bytes_remaining, flush=True)
        if Phase.count > Phase.limit:
            self.es.close()
            return 0
        nc = self.nc
        ops = self.ops
        es = self.es
        per_eng = {e: [o for o in ops if o.eng == e] for e in ENGS}
        for e in ENGS:
            comp = [o for o in per_eng[e] if not o.isdma]
            if comp:
                comp[-1].signal = True
        for o in ops:
            for d in o.deps:
                p = ops[d]
                if p.isdma:
                    continue
                if p.eng == "pe" and o.eng == "pe" and not o.isdma:
                    continue
                p.signal = True
        if Phase.gsem is None:
            Phase.gsem = ({e: nc.alloc_semaphore(f"g_e_{e}") for e in ENGS},
                          {c: nc.alloc_semaphore(f"g_c_{c}") for c in range(self.nchan)},
                          nc.alloc_semaphore("g_fin"))
            Phase.gcnt = ({e: 0 for e in ENGS}, {c: 0 for c in range(self.nchan)})
        esem, csem_all, fin = Phase.gsem
        chans = sorted({o.chan for o in ops if o.isdma})
        csem = {c: csem_all[c] for c in chans}
        fin_target = len(ENGS) * Phase.count
        cnt, ccnt = Phase.gcnt
        base = dict(cnt)
        for o in ops:
            if o.isdma:
                ccnt[o.chan] += 16
                o.sigval = ccnt[o.chan]
            elif o.signal:
                cnt[o.eng] += 1
                o.sigval = cnt[o.eng]

        def run(eng_name, e):
            waited = {}
            for o in per_eng[eng_name]:
                need = {}
                for d in o.deps:
                    p = ops[d]
                    if p.isdma:
                        key = ("c", p.chan)
                        sem = csem[p.chan]
                    else:
                        if p.eng == "pe" and o.eng == "pe" and not o.isdma:
                            continue
                        key = ("e", p.eng)
                        sem = esem[p.eng]
                    if p.sigval > need.get(key, (None, 0))[1]:
                        need[key] = (sem, p.sigval)
                for key, (sem, val) in need.items():
                    if waited.get(key, 0) >= val:
                        continue
                    e.wait_ge(sem, val)
                    waited[key] = val
                    if Phase.trace is not None:
                        Phase.trace.append((self.name, eng_name, "wait", key, val, o.idx))
                if Phase.trace is not None:
                    Phase.trace.append((self.name, eng_name, "op", o.idx, o.sigval if (o.signal or o.isdma) else None, o.chan))
                ins = o.fn(e)
                if o.isdma:
                    ins.then_inc(csem[o.chan], 16)
                elif o.signal:
                    ins.then_inc(esem[o.eng], 1)
            for c in chans:
                mine = [o for o in per_eng[eng_name] if o.isdma and o.chan == c]
                if mine and waited.get(("c", c), 0) < mine[-1].sigval:
                    e.wait_ge(csem[c], mine[-1].sigval)
            if cnt[eng_name] > base[eng_name] and waited.get(("e", eng_name), 0) < cnt[eng_name]:
                e.wait_ge(esem[eng_name], cnt[eng_name])
            e.sem_inc(fin, 1)
            e.wait_ge(fin, fin_target)

        with nc.Block() as blk:
            @blk.tensor
            def _(e):
                run("pe", e)

            @blk.scalar
            def _(e):
                run("act", e)

            @blk.vector
            def _(e):
                run("dve", e)

            @blk.gpsimd
            def _(e):
                run("pool", e)

            @blk.sync
            def _(e):
                run("sp", e)
        n = len(ops)
        self.es.close()
        return n


def build(T=4096, L=2, dbg=False, upto=99):
    NT = T // 128
    TOPK = min(256, T // 4)
    NBIS = 20
    nc = bass.Bass("TRN2", target_bir_lowering=False)
    Phase.gsem = None
    Phase.count = 0

    def din(name, shape):
        return nc.dram_tensor(name, list(shape), F32, kind="ExternalInput").ap()

    x_in = din("x", [T, D])
    norm_mix_g = din("norm_mix_g", [L, D])
    w_in = din("w_in", [L, D, IN_COLS])
    b_gate = din("b_gate", [L, 3 * D])
    conv_w = din("mlstm_conv_w", [L, 4, D])
    conv_b = din("mlstm_conv_b", [L, D])
    gate_b = din("mlstm_gate_b", [L, 8])
    mnorm_g = din("mlstm_norm_g", [L, 512])
    kvnorm_g = din("dsa_kv_norm_g", [L, 256])
    w_kv_up = din("dsa_w_kv_up", [L, 256, 1024])
    sq_g = din("dsa_q_norm_g", [L, 64])
    sk_g = din("dsa_k_norm_g", [L, 64])
    dq_g = din("diff_q_norm_g", [L, 64])
    dk_g = din("diff_k_norm_g", [L, 64])
    dlam = din("diff_lambda", [L, 256])
    dout_g = din("diff_out_norm_g", [L, 128])
    w_branch = din("w_branch", [L, 3, 512, D])
    w_out = din("w_out", [L, D, D])
    norm_ffn_g = din("norm_ffn_g", [L, D])
    w_gate_up = din("w_gate_up", [L, D, 2 * FF])
    w_down = din("w_down", [L, FF, D])
    rope_cs = din("rope_cs", [T, 64])
    out = nc.dram_tensor("out", [T, D], F32, kind="ExternalOutput").ap()

    skind = "ExternalOutput" if dbg else "Internal"

    def scr(name, shape, dt):
        return nc.dram_tensor(name, list(shape), dt, kind=skind).ap()

    xres1 = scr("xres1", [T, D], F32)
    x1 = scr("x1", [T, D], F32)
    xn2T = scr("xn2T", [D, T], BF16)
    mqkT = scr("mqkT", [1024, T], BF16)
    mv = scr("mv", [T, 512], BF16)
    mo = scr("mo", [T, 512], BF16)
    mif = scr("mif", [T, 8], F32)
    sqT = scr("sqT", [512, T], BF16)
    skT = scr("skT", [512, T], BF16)
    sv = scr("sv", [T, 512], BF16)
    iqT = scr("iqT", [256, T], F32)
    ikT = scr("ikT", [64, T], F32)
    iw = scr("iw", [T, 4], F32)
    dqT = scr("dqT", [512, T], BF16)
    dkT = scr("dkT", [512, T], BF16)
    dv = scr("dv", [T, 512], BF16)
    gates = scr("gates", [3 * D, T], BF16)
    yaT = scr("yaT", [512, T], BF16)
    ybT = scr("ybT", [512, T], BF16)
    ycT = scr("ycT", [512, T], BF16)

    top = ExitStack()

    def galloc(name, shape, dt):
        return top.enter_context(nc.sbuf_tensor(name, list(shape), dt))

    ident_f = galloc("ident_f", [128, 128], F32)
    ident_b = galloc("ident_b", [128, 128], BF16)
    tri_f = galloc("tri_f", [128, 128], F32)
    tri_b = galloc("tri_b", [128, 128], BF16)
    ones_f = galloc("ones_f", [128, 128], F32)
    mb_b = galloc("mb_b", [128, 128], BF16)
    mbq_f = galloc("mbq_f", [128, 128], F32)
    cs_all = galloc("cs_all", [128, NT, 64], F32)
    zero_c = galloc("zero_c", [128, 1], F32)
    one_c = galloc("one_c", [128, 1], F32)
    eps_c = galloc("eps_c", [128, 1], F32)

    ph = Phase(nc, "s0")
    tmpz = ph.alloc("tmpz", [128, 128], F32)
    ph.op("pool", lambda e: e.memset(ones_f[:], 1.0), writes=["ones"])
    ph.op("pool", lambda e: e.memset(tmpz[:], 0.0), writes=["tmpz"])
    ph.op("pool", lambda e: e.memset(zero_c[:], 0.0), writes=["zc"])
    ph.op("pool", lambda e: e.memset(one_c[:], 1.0), writes=["oc"])
    ph.op("pool", lambda e: e.memset(eps_c[:], EPS), writes=["ec"])
    ph.op("pool", lambda e: e.affine_select(out=ident_f[:], in_=ones_f[:], pattern=[[-1, 128]],
                                            compare_op=ALU.is_equal, fill=0.0, base=0, channel_multiplier=1),
          reads=["ones"], writes=["identf"])
    ph.op("pool", lambda e: e.affine_select(out=tri_f[:], in_=ones_f[:], pattern=[[1, 128]],
                                            compare_op=ALU.is_ge, fill=0.0, base=0, channel_multiplier=-1),
          reads=["ones"], writes=["trif"])
    ph.op("pool", lambda e: e.affine_select(out=mb_b[:], in_=tmpz[:], pattern=[[1, 128]],
                                            compare_op=ALU.is_ge, fill=-30000.0, base=0, channel_multiplier=-1),
          reads=["tmpz"], writes=["mbb"])
    ph.op("pool", lambda e: e.affine_select(out=mbq_f[:], in_=tmpz[:], pattern=[[-1, 128]],
                                            compare_op=ALU.is_ge, fill=-1e30, base=0, channel_multiplier=1),
          reads=["tmpz"], writes=["mbq"])
    ph.op("dve", lambda e: e.tensor_copy(out=ident_b[:], in_=ident_f[:]), reads=["identf"], writes=["identb"])
    ph.op("dve", lambda e: e.tensor_copy(out=tri_b[:], in_=tri_f[:]), reads=["trif"], writes=["trib"])
    ph.dma("sp", cs_all[:], rope_cs.rearrange("(n p) c -> p n c", p=128), writes=["cs"])
    ph.emit()

    def rope(ph, tag, xin, o, H, i, rkey, wkey, ts=0):
        c = cs_all[:, i, 0:32].unsqueeze(1).to_broadcast([128, H, 32])
        s = cs_all[:, i, 32:64].unsqueeze(1).to_broadcast([128, H, 32])
        tmp = ph.ropetmp[ts]
        t1, t2, t3, t4 = (tmp[k][:, 0:H, :] for k in range(4))
        R1, R2, R3, R4 = ("rt%d_%d" % (k, ts) for k in range(1, 5))
        x1a, x2a = xin[:, :, 0:32], xin[:, :, 32:64]
        ph.op("dve", lambda e: e.tensor_tensor(out=t1, in0=x1a, in1=c, op=ALU.mult), reads=[rkey], writes=[R1])
        ph.op("dve", lambda e: e.tensor_tensor(out=t2, in0=x2a, in1=s, op=ALU.mult), reads=[rkey], writes=[R2])
        ph.op("dve", lambda e: e.tensor_tensor(out=t3, in0=x1a, in1=s, op=ALU.mult), reads=[rkey], writes=[R3])
        ph.op("dve", lambda e: e.tensor_tensor(out=t4, in0=x2a, in1=c, op=ALU.mult), reads=[rkey], writes=[R4])
        ph.op("pool", lambda e: e.tensor_tensor(out=o[:, :, 0:32], in0=t1, in1=t2, op=ALU.subtract),
              reads=[R1, R2], writes=[wkey + "a"])
        ph.op("pool", lambda e: e.tensor_tensor(out=o[:, :, 32:64], in0=t3, in1=t4, op=ALU.add),
              reads=[R3, R4], writes=[wkey + "b"])

    def load_vec_cols(ph, tile_ap, vec_ap, key):
        ph.dma("sp", tile_ap, vec_ap.rearrange("(c p) -> p c", p=128), writes=[key], slow=True)

    def bcast_rows(ph, tile_ap, vec_ap, key):
        n = vec_ap.shape[0]
        ph.dma("sp", tile_ap, vec_ap.unsqueeze(0).to_broadcast([128, n]), writes=[key])

    for li in range(L):
        if li >= upto:
            break
        xsrc = x_in if li == 0 else xres1
        xdst = out if li == L - 1 else xres1
        lam_init = 0.8 - 0.6 * math.exp(-0.3 * li)
        lay = ExitStack()
        xnT = lay.enter_context(nc.sbuf_tensor(f"xnT{li}", [128, 8, T], BF16))
        gmix = lay.enter_context(nc.sbuf_tensor(f"gmix{li}", [128, 8], F32))

        ph = Phase(nc, f"a1_{li}")
        ph.ropetmp = [[ph.alloc(f"rt{k}", [128, 8, 32], F32) for k in range(4)]]
        widx = ph.alloc("widx", [128, 8, 324], F32)
        load_vec_cols(ph, gmix[:], norm_mix_g[li], "gmix")
        ph.dma("sp", widx[:], w_in[li][:, 2824:3148].rearrange("(c p) n -> p c n", p=128), writes=["widx"])
        ph.op("dve", lambda e: e.tensor_tensor(out=widx[:], in0=widx[:],
                                               in1=gmix[:].unsqueeze(2).to_broadcast([128, 8, 324]), op=ALU.mult),
              reads=["widx", "gmix"], writes=["widx"])
        xt = [ph.alloc(f"xt{k}", [128, D], F32) for k in range(2)]
        junk = ph.alloc("junk", [128, D], BF16)
        ss = [ph.alloc(f"ss{k}", [128, 1], F32) for k in range(2)]
        xh = [ph.alloc(f"xh{k}", [128, D], F32) for k in range(2)]
        xhT = [ph.alloc(f"xhT{k}", [128, 8, 128], F32) for k in range(2)]
        zi = [ph.alloc(f"zi{k}", [128, 324], F32) for k in range(2)]
        zr = [ph.alloc(f"zr{k}", [128, 5, 64], F32) for k in range(2)]
        zT = [ph.alloc(f"zT{k}", [128, 3, 128], F32) for k in range(2)]
        iwt = [ph.alloc(f"iwt{k}", [128, 4], F32) for k in range(2)]
        pT = [ph.psum(f"pT{k}", [128, D], F32) for k in range(2)]
        pI = [ph.psum(f"pI{k}", [128, 324], F32) for k in range(2)]
        pZ = ph.psum("pZ", [128, 3, 128], F32)
        for i in range(NT):
            k = i % 2
            K = str(k)
            tsl = slice(i * 128, (i + 1) * 128)
            ph.dma("sp", xt[k][:], xsrc[tsl, :], writes=["xt" + K])
            ph.op("act", lambda e, k=k: e.activation(out=junk[:], in_=xt[k][:], func=AF.Square, accum_out=ss[k][:]),
                  reads=["xt" + K], writes=["junk", "ss" + K])
            ph.op("act", lambda e, k=k: e.activation(out=ss[k][:], in_=ss[k][:], func=AF.Sqrt, bias=eps_c[:, 0:1], scale=1.0 / D),
                  reads=["ss" + K], writes=["ss" + K])
            ph.op("dve", lambda e, k=k: e.reciprocal(out=ss[k][:], in_=ss[k][:]), reads=["ss" + K], writes=["ss" + K])
            ph.op("dve", lambda e, k=k: e.tensor_scalar(out=xh[k][:], in0=xt[k][:], scalar1=ss[k][:, 0:1], scalar2=None,
                                                        op0=ALU.mult), reads=["xt" + K, "ss" + K], writes=["xh" + K])
            for c in range(8):
                ph.op("pe", lambda e, k=k, c=c: e.transpose(pT[k][:, c * 128:(c + 1) * 128], xh[k][:, c * 128:(c + 1) * 128],
                                                            ident_f[:]), reads=["xh" + K], writes=["pT" + K])
            ph.op("act", lambda e, k=k, tsl=tsl: e.activation(out=xnT[:, :, tsl], in_=pT[k][:].rearrange("p (c t) -> p c t", c=8),
                                                              func=AF.Copy), reads=["pT" + K], writes=[("xnT", i)])
            for hb2 in range(2):
                ph.op("dve", lambda e, k=k, hb2=hb2: e.tensor_copy(out=xhT[k][:, hb2 * 4:(hb2 + 1) * 4, :],
                                                                  in_=pT[k][:, hb2 * 512:(hb2 + 1) * 512].rearrange("p (c t) -> p c t", c=4)),
                      reads=["pT" + K], writes=["xhT" + K])
            for c in range(8):
                ph.op("pe", lambda e, k=k, c=c: e.matmul(pI[k][:], lhsT=xhT[k][:, c, :], rhs=widx[:, c, :],
                                                         start=(c == 0), stop=(c == 7)),
                      reads=["xhT" + K, "widx"], writes=["pI" + K])
            ph.op("act", lambda e, k=k: e.activation(out=zi[k][:], in_=pI[k][:], func=AF.Copy), reads=["pI" + K], writes=["zi" + K])
            rope(ph, "i", zi[k][:, 0:320].rearrange("p (h d) -> p h d", d=64), zr[k][:], 5, i, "zi" + K, "zr" + K)
            ph.op("pool", lambda e, k=k: e.tensor_scalar(out=iwt[k][:], in0=zi[k][:, 320:324], scalar1=1.0 / 16.0, scalar2=None,
                                                         op0=ALU.mult), reads=["zi" + K], writes=["iwt" + K])
            ph.dma("act", iw[tsl, :], iwt[k][:], reads=["iwt" + K], writes=[("iw", i)])
            zr2 = zr[k][:].rearrange("p h d -> p (h d)")
            for c in range(2):
                ph.op("pe", lambda e, c=c, zr2=zr2: e.transpose(pZ[:, c, :], zr2[:, c * 128:(c + 1) * 128], ident_f[:]),
                      reads=["zr" + K + "a", "zr" + K + "b"], writes=["pZ"])
            ph.op("pe", lambda e, zr2=zr2: e.transpose(pZ[0:64, 2, :], zr2[:, 256:320], ident_f[:]),
                  reads=["zr" + K + "a", "zr" + K + "b"], writes=["pZ"])
            ph.op("act", lambda e, k=k: e.activation(out=zT[k][:, 0:2, :], in_=pZ[:, 0:2, :], func=AF.Copy),
                  reads=["pZ"], writes=["zTa" + K])
            ph.op("dve", lambda e, k=k: e.tensor_copy(out=zT[k][0:64, 2, :], in_=pZ[0:64, 2, :]), reads=["pZ"], writes=["zTb" + K])
            ph.dma("act", iqT[:, tsl].rearrange("(c p) t -> p c t", p=128), zT[k][:, 0:2, :], reads=["zTa" + K], writes=[("iqT", i)])
            ph.dma("act", ikT[:, tsl], zT[k][0:64, 2, :], reads=["zTb" + K], writes=[("ikT", i)])
        ph.emit()

        ph = Phase(nc, f"a2_{li}")
        ph.ropetmp = [[ph.alloc(f"rt{k}_{v}", [128, 8, 32], F32) for k in range(4)] for v in range(2)]
        wst = [ph.alloc(f"wst{k}", [128, 8, 512], F32) for k in range(2)]
        wb = [ph.alloc(f"wb{k}", [128, 8, 512], BF16) for k in range(2)]
        wkvs = ph.alloc("wkvs", [128, 2, 1024], F32)
        wkv = ph.alloc("wkv", [128, 2, 1024], BF16)
        kvg = ph.alloc("kvg", [128, 2], F32)
        gtile = {}
        for nm, src, sc in (("sq", sq_g, 0.125), ("sk", sk_g, 1.0), ("dq", dq_g, 0.125), ("dk", dk_g, 1.0)):
            gtile[nm] = ph.alloc("g_" + nm, [128, 64], F32)
            bcast_rows(ph, gtile[nm][:], src[li], "g_" + nm)
            if sc != 1.0:
                ph.op("pool", lambda e, t=gtile[nm], sc=sc: e.tensor_scalar(out=t[:], in0=t[:], scalar1=sc, scalar2=None, op0=ALU.mult),
                      reads=["g_" + nm], writes=["g_" + nm])
        gbt = ph.alloc("gbt", [128, 8], F32)
        bcast_rows(ph, gbt[:], gate_b[li], "gbt")
        load_vec_cols(ph, kvg[:], kvnorm_g[li], "kvg")
        ph.dma("sp", wkvs[:], w_kv_up[li].rearrange("(c p) n -> p c n", p=128), writes=["wkvs"])
        ph.op("dve", lambda e: e.tensor_tensor(out=wkv[:], in0=wkvs[:], in1=kvg[:].unsqueeze(2).to_broadcast([128, 2, 1024]),
                                               op=ALU.mult), reads=["wkvs", "kvg"], writes=["wkv"])
        pa = [ph.psum(f"pa{k}", [128, 512], F32) for k in range(2)]
        pkv = [ph.psum(f"pkv{k}", [128, 512], F32) for k in range(2)]
        pTb = ph.psum("pTb", [128, 4, 128], BF16)
        pC2 = ph.psum("pC2", [128, 2, 128], BF16)
        ob = [ph.alloc(f"ob{k}", [128, 512], BF16) for k in range(2)]
        sq2s = [ph.alloc(f"sq2{v}", [128, 512], F32) for v in range(2)]
        ssqs = [ph.alloc(f"ssq{v}", [128, 8], F32) for v in range(2)]
        xn_s = [ph.alloc(f"xn_{v}", [128, 8, 64], F32) for v in range(2)]
        xrs = [ph.alloc(f"xr{v}", [128, 8, 64], BF16) for v in range(2)]
        sq2 = sq2s[0]
        oT = [ph.alloc(f"oT{k}", [128, 4, 128], BF16) for k in range(2)]
        s1 = ph.alloc("s1", [128, 1], F32)
        ckn = ph.alloc("ckn", [128, 256], BF16)
        ckT = ph.alloc("ckT", [128, 2, 128], BF16)
        zf = [ph.alloc(f"zf{k}", [128, 8], F32) for k in range(2)]
        ef = ph.alloc("ef", [128, 4], F32)
        cnt_hn = [0]

        def headnorm_rope_T(psrc, pkey, gname, dstT, i):
            tsl = slice(i * 128, (i + 1) * 128)
            k = cnt_hn[0] % 2
            cnt_hn[0] += 1
            sq2v, ssq, xn_, xr = sq2s[k], ssqs[k], xn_s[k], xrs[k]
            SQ, SS, XN, XR = "sq2%d" % k, "ssq%d" % k, "xn_%d" % k, "xr%d" % k
            ph.op("act", lambda e: e.activation(out=sq2v[:], in_=psrc, func=AF.Square), reads=[pkey], writes=[SQ])
            ph.op("dve", lambda e: e.tensor_reduce(out=ssq[:], in_=sq2v[:].rearrange("p (h d) -> p h d", d=64), axis=AX.X, op=ALU.add),
                  reads=[SQ], writes=[SS])
            ph.op("act", lambda e: e.activation(out=ssq[:], in_=ssq[:], func=AF.Sqrt, bias=eps_c[:, 0:1], scale=1.0 / 64), reads=[SS], writes=[SS])
            ph.op("dve", lambda e: e.reciprocal(out=ssq[:], in_=ssq[:]), reads=[SS], writes=[SS])
            ph.op("dve", lambda e: e.tensor_tensor(out=xn_[:], in0=psrc.rearrange("p (h d) -> p h d", d=64),
                                                   in1=ssq[:].unsqueeze(2).to_broadcast([128, 8, 64]), op=ALU.mult),
                  reads=[pkey, SS], writes=[XN])
            ph.op("pool", lambda e: e.tensor_tensor(out=xn_[:], in0=xn_[:], in1=gtile[gname][:].unsqueeze(1).to_broadcast([128, 8, 64]),
                                                    op=ALU.mult), reads=[XN, "g_" + gname], writes=[XN])
            rope(ph, gname, xn_[:], xr[:], 8, i, XN, XR, ts=k)
            xr2 = xr[:].rearrange("p h d -> p (h d)")
            for c in range(4):
                ph.op("pe", lambda e, c=c: e.transpose(pTb[:, c, :], xr2[:, c * 128:(c + 1) * 128], ident_b[:]),
                      reads=[XR + "a", XR + "b"], writes=["pTb"])
            ph.op("act", lambda e, k=k: e.activation(out=oT[k][:], in_=pTb[:], func=AF.Copy), reads=["pTb"], writes=["oT%d" % k])
            ph.dma("act", dstT[:, tsl].rearrange("(c p) t -> p c t", p=128), oT[k][:], reads=["oT%d" % k], writes=[("dstT", gname, i)])

        blocks = [("mv", 1024, 512), ("mo", 1536, 512), ("mif", 2048, 8), ("sq", 2056, 512), ("ckv", 2568, 256),
                  ("dq", 3148, 512), ("dk", 3660, 512), ("dv", 4172, 512)]
        cnt_ob = 0
        for bi, (bn, c0, n) in enumerate(blocks):
            w = bi % 2
            W = str(w)
            for hh in range(2):
                ph.dma("sp", wst[w][:, hh * 4:(hh + 1) * 4, 0:n],
                       w_in[li][hh * 512:(hh + 1) * 512, c0:c0 + n].rearrange("(c p) n -> p c n", p=128),
                       writes=["wst" + W + str(hh)])
            ph.op("dve" if bi % 2 == 0 else "pool",
                  lambda e, w=w, n=n: e.tensor_tensor(out=wb[w][:, :, 0:n], in0=wst[w][:, :, 0:n],
                                                      in1=gmix[:].unsqueeze(2).to_broadcast([128, 8, n]), op=ALU.mult),
                  reads=["wst" + W + "0", "wst" + W + "1", "gmix"], writes=["wb" + W])
            for i in range(NT):
                tsl = slice(i * 128, (i + 1) * 128)
                p = i % 2
                P = "pa%d" % p
                for c in range(8):
                    ph.op("pe", lambda e, w=w, n=n, p=p, c=c, tsl=tsl: e.matmul(pa[p][:, 0:n], lhsT=xnT[:, c, tsl], rhs=wb[w][:, c, 0:n],
                                                                                start=(c == 0), stop=(c == 7)),
                          reads=["wb" + W], writes=[P])
                if bn in ("mv", "mo", "dv"):
                    o = cnt_ob % 2
                    cnt_ob += 1
                    fn = AF.Sigmoid if bn == "mo" else AF.Copy
                    dst = {"mv": mv, "mo": mo, "dv": dv}[bn]
                    ph.op("act", lambda e, o=o, p=p, fn=fn: e.activation(out=ob[o][:], in_=pa[p][:], func=fn), reads=[P], writes=["ob%d" % o])
                    ph.dma("act", dst[tsl, :], ob[o][:], reads=["ob%d" % o], writes=[(bn, i)])
                elif bn in ("sq", "dq", "dk"):
                    headnorm_rope_T(pa[p][:], P, bn, {"sq": sqT, "dq": dqT, "dk": dkT}[bn], i)
                elif bn == "mif":
                    z = i % 2
                    Z = "zf%d" % z
                    ph.op("dve", lambda e, z=z, p=p: e.tensor_tensor(out=zf[z][:], in0=pa[p][:, 0:8], in1=gbt[:], op=ALU.add),
                          reads=[P, "gbt"], writes=[Z])
                    ph.op("act", lambda e, z=z: e.activation(out=ef[:], in_=zf[z][:, 4:8], func=AF.Exp, scale=-1.0), reads=[Z], writes=["ef"])
                    ph.op("act", lambda e: e.activation(out=ef[:], in_=ef[:], func=AF.Ln, bias=one_c[:, 0:1]), reads=["ef"], writes=["ef"])
                    ph.op("dve", lambda e, z=z: e.tensor_scalar(out=zf[z][:, 4:8], in0=ef[:], scalar1=-1.0, scalar2=None, op0=ALU.mult),
                          reads=["ef", Z], writes=[Z])
                    ph.dma("act", mif[tsl, :], zf[z][:], reads=[Z], writes=[("mif", i)])
                elif bn == "ckv":
                    ph.op("act", lambda e, p=p: e.activation(out=sq2[:, 0:256], in_=pa[p][:, 0:256], func=AF.Square, accum_out=s1[:]),
                          reads=[P], writes=["sq20", "s1"])
                    ph.op("act", lambda e: e.activation(out=s1[:], in_=s1[:], func=AF.Sqrt, bias=eps_c[:, 0:1], scale=1.0 / 256), reads=["s1"], writes=["s1"])
                    ph.op("dve", lambda e: e.reciprocal(out=s1[:], in_=s1[:]), reads=["s1"], writes=["s1"])
                    ph.op("dve", lambda e, p=p: e.tensor_scalar(out=ckn[:], in0=pa[p][:, 0:256], scalar1=s1[:, 0:1], scalar2=None, op0=ALU.mult),
                          reads=[P, "s1"], writes=["ckn"])
                    for c in range(2):
                        ph.op("pe", lambda e, c=c: e.transpose(pC2[:, c, :], ckn[:, c * 128:(c + 1) * 128], ident_b[:]), reads=["ckn"], writes=["pC2"])
                    ph.op("act", lambda e: e.activation(out=ckT[:], in_=pC2[:], func=AF.Copy), reads=["pC2"], writes=["ckT"])
                    for hf in range(2):
                        for c in range(2):
                            ph.op("pe", lambda e, hf=hf, c=c: e.matmul(pkv[hf][:], lhsT=ckT[:, c, :], rhs=wkv[:, c, hf * 512:(hf + 1) * 512],
                                                                       start=(c == 0), stop=(c == 1)),
                                  reads=["ckT", "wkv"], writes=["pkv%d" % hf])
                    headnorm_rope_T(pkv[0][:], "pkv0", "sk", skT, i)
                    o = cnt_ob % 2
                    cnt_ob += 1
                    ph.op("act", lambda e, o=o: e.activation(out=ob[o][:], in_=pkv[1][:], func=AF.Copy), reads=["pkv1"], writes=["ob%d" % o])
                    ph.dma("act", sv[tsl, :], ob[o][:], reads=["ob%d" % o], writes=[("sv", i)])
        ph.emit()

        ph = Phase(nc, f"a3_{li}")
        w3s = [ph.alloc(f"w3s{k}", [128, 8, 128], F32) for k in range(2)]
        w3 = [ph.alloc(f"w3{k}", [128, 8, 128], BF16) for k in range(2)]
        cw = ph.alloc("cw", [128, 8, 4], F32)
        cb = ph.alloc("cb", [128, 8], F32)
        bg = ph.alloc("bg", [128, 24], F32)
        for j in range(4):
            ph.dma("sp", cw[:, :, j], conv_w[li, j].rearrange("(c p) -> p c", p=128), reads=["cwx"] if j else [], writes=["cw"], slow=True)
        load_vec_cols(ph, cb[:], conv_b[li], "cb")
        load_vec_cols(ph, bg[:], b_gate[li], "bg")
        zc = [ph.alloc(f"zc{k}", [128, T + 3], F32) for k in range(2)]
        acc = ph.alloc("acc", [128, T], F32)
        o3 = [ph.alloc(f"o3{k}", [128, T], BF16) for k in range(2)]
        p3 = [ph.psum(f"p3{k}", [128, 512], F32) for k in range(2)]
        for k in range(2):
            ph.op("pool", lambda e, k=k: e.memset(zc[k][:, 0:3], 0.0), writes=["zc%d" % k])
        NTB = T // 512
        for fc in range(32):
            w = fc % 2
            W = str(w)
            col0 = fc * 128 if fc < 8 else 4684 + (fc - 8) * 128
            ph.dma("sp", w3s[w][:], w_in[li][:, col0:col0 + 128].rearrange("(c p) n -> p c n", p=128), writes=["w3s" + W])
            ph.op("dve" if fc % 2 == 0 else "pool",
                  lambda e, w=w: e.tensor_tensor(out=w3[w][:], in0=w3s[w][:], in1=gmix[:].unsqueeze(2).to_broadcast([128, 8, 128]), op=ALU.mult),
                  reads=["w3s" + W, "gmix"], writes=["w3" + W])
            for tb in range(NTB):
                p = (fc * NTB + tb) % 2
                P = "p3%d" % p
                bsl = slice(tb * 512, (tb + 1) * 512)
                for c in range(8):
                    ph.op("pe", lambda e, w=w, p=p, c=c, bsl=bsl: e.matmul(p3[p][:], lhsT=w3[w][:, c, :], rhs=xnT[:, c, bsl],
                                                                           start=(c == 0), stop=(c == 7)),
                          reads=["w3" + W], writes=[P])
                if fc >= 8:
                    ph.op("act", lambda e, w=w, p=p, bsl=bsl, fc=fc: e.activation(out=o3[w][:, bsl], in_=p3[p][:], func=AF.Sigmoid,
                                                                                  bias=bg[:, fc - 8:fc - 7]),
                          reads=[P, "bg"], writes=["o3" + W])
                else:
                    ph.op("act", lambda e, w=w, p=p, tb=tb: e.activation(out=zc[w][:, 3 + tb * 512:3 + (tb + 1) * 512], in_=p3[p][:], func=AF.Copy),
                          reads=[P], writes=["zc" + W])
            if fc >= 8:
                ph.dma("act", gates[(fc - 8) * 128:(fc - 7) * 128, :], o3[w][:], reads=["o3" + W], writes=[("gates", fc)])
            else:
                ph.op("dve", lambda e, w=w, fc=fc: e.tensor_scalar(out=acc[:], in0=zc[w][:, 0:T], scalar1=cw[:, fc, 0:1], scalar2=None, op0=ALU.mult),
                      reads=["zc" + W, "cw"], writes=["acc"])
                for j in range(1, 4):
                    ph.op("dve", lambda e, w=w, fc=fc, j=j: e.scalar_tensor_tensor(out=acc[:], in0=zc[w][:, j:j + T], scalar=cw[:, fc, j:j + 1],
                                                                                   in1=acc[:], op0=ALU.mult, op1=ALU.add),
                          reads=["zc" + W, "cw", "acc"], writes=["acc"])
                ph.op("act", lambda e, w=w, fc=fc: e.activation(out=o3[w][:], in_=acc[:], func=AF.Silu, bias=cb[:, fc:fc + 1]),
                      reads=["acc", "cb"], writes=["o3" + W])
                ph.dma("act", mqkT[fc * 128:(fc + 1) * 128, :], o3[w][:], reads=["o3" + W], writes=[("mqkT", fc)])
        ph.emit()
        lay.close()
        if li * 10 + 1 >= upto * 10 + (upto % 1):
            pass

        ph = Phase(nc, f"b1_{li}")
        qk = [ph.alloc(f"qk{k}", [128, 8, 128], BF16) for k in range(2)]
        vp = [ph.alloc(f"vp{k}", [128, 4, 129], BF16) for k in range(2)]
        mft = [ph.alloc(f"mft{k}", [128, 8], F32) for k in range(2)]
        mot = [ph.alloc(f"mot{k}", [128, 512], BF16) for k in range(2)]
        mg = ph.alloc("mg", [128, 512], F32)
        bcast_rows(ph, mg[:], mnorm_g[li], "mg")
        GO = ph.alloc("GO", [128, 512], F32)
        bs = ph.alloc("bs", [128, 8], F32)
        ebt = ph.alloc("ebt", [128, 4], F32)
        apre = ph.alloc("apre", [128, 4], F32)
        a_s = ph.alloc("a_s", [128, 4], F32)
        wpre = ph.alloc("wpre", [128, 4], F32)
        wstt = ph.alloc("wstt", [128, 4], F32)
        eg = ph.alloc("eg", [128, 4], F32)
        lnsc = ph.alloc("lnsc", [128, 1], F32)
        ph.op("pool", lambda e: e.memset(lnsc[:], -0.5 * math.log(128.0)), writes=["lnsc"])
        epsc = ph.alloc("epsc", [128, 1], F32)
        ph.op("pool", lambda e: e.memset(epsc[:], EPS), writes=["epsc"])
        ST = [ph.alloc(f"ST{k}", [128, 128], BF16) for k in range(2)]
        kw = [ph.alloc(f"kw{k}", [128, 128], BF16) for k in range(2)]
        CTf = [ph.alloc(f"CTf{h}", [128, 129], F32) for h in range(4)]
        CTb = [ph.alloc(f"CTb{h}", [128, 129], BF16) for h in range(4)]
        dns = [ph.alloc(f"dn{h}", [128, 1], F32) for h in range(4)]
        facs = [ph.alloc(f"fac{h}", [128, 1], F32) for h in range(4)]
        ss1s = [ph.alloc(f"ss1{h}", [128, 1], F32) for h in range(4)]
        junk1s = [ph.alloc(f"junk1{h}", [128, 128], BF16) for h in range(4)]
        ya = [ph.alloc(f"ya{k}", [128, 512], BF16) for k in range(2)]
        yT = [ph.alloc(f"yT{k}", [128, 4, 128], BF16) for k in range(2)]
        pB = ph.psum("pB", [128, 8], F32)
        pS = [ph.psum(f"pS{k}", [128, 128], F32) for k in range(2)]
        pK = ph.psum("pK", [128, 128], BF16)
        pO = [ph.psum(f"pO{k}", [128, 129], F32) for k in range(2)]
        pC = ph.psum("pC", [128, 129], F32)
        pY = ph.psum("pY", [128, 4, 128], BF16)
        for k in range(2):
            ph.op("pool", lambda e, k=k: e.memset(vp[k][:], 1.0), writes=["vp%d" % k])
        for h in range(4):
            ph.op("pool", lambda e, h=h: e.memset(CTf[h][:], 0.0), writes=["CTf%d" % h])
            ph.op("pool", lambda e, h=h: e.memset(CTb[h][:], 0.0), writes=["CTb%d" % h])
        for n in range(NT):
            k = n % 2
            K = str(k)
            tsl = slice(n * 128, (n + 1) * 128)
            ph.dma("sp", qk[k][:], mqkT[:, tsl].rearrange("(g p) t -> p g t", p=128), writes=["qk" + K])
            ph.dma("sp", vp[k][:, :, 0:128], mv[tsl, :].rearrange("t (h v) -> t h v", v=128), writes=["vp" + K])
            ph.dma("sp", mft[k][:], mif[tsl, :], writes=["mft" + K])
            ph.dma("sp", mot[k][:], mo[tsl, :], writes=["mot" + K])
            ph.op("pe", lambda e, k=k: e.matmul(pB[:, 0:4], lhsT=tri_f[:], rhs=mft[k][:, 4:8], start=True, stop=False, skip_group_check=True),
                  reads=["mft" + K], writes=["pB"])
            ph.op("pe", lambda e, k=k: e.matmul(pB[:, 4:8], lhsT=ones_f[:], rhs=mft[k][:, 4:8], start=False, stop=True, skip_group_check=True),
                  reads=["mft" + K], writes=["pB"])
            ph.op("dve", lambda e: e.tensor_copy(out=bs[:], in_=pB[:]), reads=["pB"], writes=["bs"])
            ph.op("act", lambda e: e.activation(out=ebt[:], in_=bs[:, 0:4], func=AF.Exp), reads=["bs"], writes=["ebt"])
            ph.op("act", lambda e: e.activation(out=eg[:], in_=bs[:, 4:8], func=AF.Exp), reads=["bs"], writes=["eg"])
            ph.op("dve", lambda e, k=k: e.tensor_tensor(out=apre[:], in0=mft[k][:, 0:4], in1=bs[:, 0:4], op=ALU.subtract),
                  reads=["mft" + K, "bs"], writes=["apre"])
            ph.op("act", lambda e: e.activation(out=a_s[:], in_=apre[:], func=AF.Exp, bias=lnsc[:, 0:1]), reads=["apre", "lnsc"], writes=["a_s"])
            ph.op("dve", lambda e: e.tensor_tensor(out=wpre[:], in0=apre[:], in1=bs[:, 4:8], op=ALU.add), reads=["apre", "bs"], writes=["wpre"])
            ph.op("act", lambda e: e.activation(out=wstt[:], in_=wpre[:], func=AF.Exp, bias=lnsc[:, 0:1]), reads=["wpre", "lnsc"], writes=["wstt"])
            ph.op("pool", lambda e, k=k: e.tensor_tensor(out=GO[:], in0=mot[k][:], in1=mg[:], op=ALU.mult), reads=["mot" + K, "mg"], writes=["GO"])
            y = n % 2
            for h in range(4):
                s = (n * 4 + h) % 2
                S = str(s)
                ph.op("pe", lambda e, k=k, h=h, s=s: e.matmul(pS[s][:], lhsT=qk[k][:, 4 + h, :], rhs=qk[k][:, h, :], start=True, stop=True),
                      reads=["qk" + K], writes=["pS" + S])
                ph.op("dve", lambda e, h=h, s=s: e.scalar_tensor_tensor(out=ST[s][:], in0=pS[s][:], scalar=a_s[:, h:h + 1], in1=tri_b[:],
                                                                        op0=ALU.mult, op1=ALU.mult),
                      reads=["pS" + S, "a_s"], writes=["ST" + S])
                ph.op("pe", lambda e, k=k, h=h: e.transpose(pK[:], qk[k][:, 4 + h, :], ident_b[:]), reads=["qk" + K], writes=["pK"])
                ph.op("act", lambda e, h=h, s=s: e.activation(out=kw[s][:], in_=pK[:], func=AF.Copy, scale=wstt[:, h:h + 1]),
                      reads=["pK", "wstt"], writes=["kw" + S])
                ph.op("pe", lambda e, k=k, h=h, s=s: e.matmul(pO[s][:], lhsT=ST[s][:], rhs=vp[k][:, h, :], start=True, stop=False),
                      reads=["ST" + S, "vp" + K], writes=["pO" + S])
                ph.op("pe", lambda e, k=k, h=h, s=s: e.matmul(pO[s][:], lhsT=qk[k][:, h, :], rhs=CTb[h][:], start=False, stop=True),
                      reads=["qk" + K, "CTb%d" % h], writes=["pO" + S])
                ph.op("pe", lambda e, k=k, h=h, s=s: e.matmul(pC[:], lhsT=kw[s][:], rhs=vp[k][:, h, :], start=True, stop=True),
                      reads=["kw" + S, "vp" + K], writes=["pC"])
                ph.op("dve", lambda e, h=h: e.scalar_tensor_tensor(out=CTf[h][:], in0=CTf[h][:], scalar=eg[:, h:h + 1], in1=pC[:],
                                                                   op0=ALU.mult, op1=ALU.add),
                      reads=["CTf%d" % h, "eg", "pC"], writes=["CTf%d" % h])
                ph.op("pool", lambda e, h=h: e.tensor_copy(out=CTb[h][:], in_=CTf[h][:]), reads=["CTf%d" % h], writes=["CTb%d" % h])
                dn, fac, ss1, junk1 = dns[h], facs[h], ss1s[h], junk1s[h]
                DN, FAC, SS1, JK = "dn%d" % h, "fac%d" % h, "ss1%d" % h, "junk1%d" % h
                ph.op("act", lambda e, h=h, s=s, dn=dn, fac=fac, ss1=ss1, junk1=junk1: e.activation(out=dn[:], in_=pO[s][:, 128:129], func=AF.Abs, scale=ebt[:, h:h + 1]),
                      reads=["pO" + S, "ebt"], writes=[DN])
                ph.op("dve", lambda e, dn=dn, fac=fac, ss1=ss1, junk1=junk1: e.tensor_scalar(out=dn[:], in0=dn[:], scalar1=1.0, scalar2=None, op0=ALU.max), reads=[DN], writes=[DN])
                ph.op("dve", lambda e, dn=dn, fac=fac, ss1=ss1, junk1=junk1: e.reciprocal(out=dn[:], in_=dn[:]), reads=[DN], writes=[DN])
                ph.op("dve", lambda e, h=h, dn=dn, fac=fac, ss1=ss1, junk1=junk1: e.tensor_tensor(out=fac[:], in0=dn[:], in1=ebt[:, h:h + 1], op=ALU.mult), reads=[DN, "ebt"], writes=[FAC])
                ph.op("act", lambda e, s=s, dn=dn, fac=fac, ss1=ss1, junk1=junk1: e.activation(out=junk1[:], in_=pO[s][:, 0:128], func=AF.Square, scale=fac[:, 0:1], accum_out=ss1[:]),
                      reads=["pO" + S, FAC], writes=[JK, SS1])
                ph.op("act", lambda e, dn=dn, fac=fac, ss1=ss1, junk1=junk1: e.activation(out=ss1[:], in_=ss1[:], func=AF.Sqrt, bias=epsc[:, 0:1], scale=1.0 / 128), reads=[SS1, "epsc"], writes=[SS1])
                ph.op("dve", lambda e, dn=dn, fac=fac, ss1=ss1, junk1=junk1: e.reciprocal(out=ss1[:], in_=ss1[:]), reads=[SS1], writes=[SS1])
                ph.op("dve", lambda e, dn=dn, fac=fac, ss1=ss1, junk1=junk1: e.tensor_tensor(out=fac[:], in0=fac[:], in1=ss1[:], op=ALU.mult), reads=[FAC, SS1], writes=[FAC])
                ph.op("dve", lambda e, h=h, s=s, y=y, dn=dn, fac=fac, ss1=ss1, junk1=junk1: e.scalar_tensor_tensor(out=ya[y][:, h * 128:(h + 1) * 128], in0=pO[s][:, 0:128], scalar=fac[:, 0:1],
                                                                             in1=GO[:, h * 128:(h + 1) * 128], op0=ALU.mult, op1=ALU.mult),
                      reads=["pO" + S, FAC, "GO"], writes=[("ya", y, h)])
            for c in range(4):
                ph.op("pe", lambda e, c=c, y=y: e.transpose(pY[:, c, :], ya[y][:, c * 128:(c + 1) * 128], ident_b[:]),
                      reads=[("ya", y, c)], writes=["pY"])
            ph.op("act", lambda e, y=y: e.activation(out=yT[y][:], in_=pY[:], func=AF.Copy), reads=["pY"], writes=["yT%d" % y])
            ph.dma("act", yaT[:, tsl].rearrange("(c p) t -> p c t", p=128), yT[y][:], reads=["yT%d" % y], writes=[("yaT", n)])
        ph.emit()

        ph = Phase(nc, f"b2_{li}")
        dqs = [ph.alloc(f"dq{k}", [128, 2, T], BF16) for k in range(2)]
        for k in range(2):
            ph.op("pool", lambda e, k=k: e.memset(dqs[k][:], 0.0), writes=["dq%d" % k])
        dks = [ph.alloc(f"dk{k}", [128, T], BF16) for k in range(2)]
        dvp = [ph.alloc(f"dvp{k}", [128, NT, 129], BF16) for k in range(2)]
        lam4 = ph.alloc("lam4", [128, 256], F32)
        lprod = ph.alloc("lprod", [128, 128], F32)
        lsum = ph.alloc("lsum", [128, 2], F32)
        nlam = ph.alloc("nlam", [128, 1], F32)
        gout = ph.alloc("gout", [128, 128], F32)
        epsc = ph.alloc("epsc", [128, 1], F32)
        ph.op("pool", lambda e: e.memset(epsc[:], EPS), writes=["epsc"])
        bcast_rows(ph, lam4[:], dlam[li], "lam4")
        bcast_rows(ph, gout[:], dout_g[li], "gout")
        ph.op("pool", lambda e: e.tensor_scalar(out=gout[:], in0=gout[:], scalar1=1.0 - lam_init, scalar2=None, op0=ALU.mult),
              reads=["gout"], writes=["gout"])
        l3 = lam4[:].rearrange("p (a d) -> p a d", d=64)
        ph.op("dve", lambda e: e.tensor_tensor(out=lprod[:].rearrange("p (a d) -> p a d", d=64), in0=l3[:, 0:4:2, :], in1=l3[:, 1:4:2, :], op=ALU.mult),
              reads=["lam4"], writes=["lprod"])
        ph.op("dve", lambda e: e.tensor_reduce(out=lsum[:], in_=lprod[:].rearrange("p (a d) -> p a d", d=64), axis=AX.X, op=ALU.add),
              reads=["lprod"], writes=["lsum"])
        ph.op("act", lambda e: e.activation(out=lsum[:], in_=lsum[:], func=AF.Exp), reads=["lsum"], writes=["lsum"])
        ph.op("dve", lambda e: e.tensor_tensor(out=nlam[:], in0=lsum[:, 1:2], in1=lsum[:, 0:1], op=ALU.subtract), reads=["lsum"], writes=["nlam"])
        ph.op("dve", lambda e: e.tensor_scalar(out=nlam[:], in0=nlam[:], scalar1=-lam_init, scalar2=None, op0=ALU.add), reads=["nlam"], writes=["nlam"])
        pS2 = [ph.psum(f"pS{k}", [128, 512], F32) for k in range(2)]
        pO2 = [[ph.psum(f"pO{c}{k}", [128, 2, 129], F32) for k in range(2)] for c in range(2)]
        pY2 = ph.psum("pY", [128, 128], BF16)
        PT = [ph.alloc(f"PT{k}", [128, 512], BF16) for k in range(2)]
        r01 = ph.alloc("r01", [128, 2], F32)
        t1_ = ph.alloc("t1_", [128, 128], F32)
        o_ = ph.alloc("o_", [128, 128], F32)
        ss2 = ph.alloc("ss2", [128, 1], F32)
        junk2 = ph.alloc("junk2", [128, 128], BF16)
        yc = [ph.alloc(f"yc{k}", [128, 128], BF16) for k in range(2)]
        ycTt = [ph.alloc(f"ycT{k}", [128, 128], BF16) for k in range(2)]
        for k in range(2):
            ph.op("pool", lambda e, k=k: e.memset(dvp[k][:], 1.0), writes=["dvp%d" % k])
        cntS = 0
        cntY = 0
        NQB = T // 256
        for h in range(4):
            hb = h % 2
            HB = str(hb)
            ph.dma("sp", dqs[hb][0:64, 0, :], dqT[h * 128:h * 128 + 64, :], reads=["dq" + HB], writes=["dqa" + HB])
            ph.dma("sp", dqs[hb][64:128, 1, :], dqT[h * 128 + 64:(h + 1) * 128, :], reads=["dq" + HB], writes=["dqb" + HB])
            ph.dma("sp", dks[hb][:], dkT[h * 128:(h + 1) * 128, :], writes=["dk" + HB])
            ph.dma("sp", dvp[hb][:, :, 0:128], dv[:, h * 128:(h + 1) * 128].rearrange("(n p) v -> p n v", p=128), writes=["dvp" + HB])
            for qb in range(NQB):
                ob_ = qb % 2
                qsl = slice(qb * 256, (qb + 1) * 256)
                first = [True, True]
                for j in range(2 * qb + 2):
                    s = cntS % 2
                    cntS += 1
                    S = str(s)
                    jsl = slice(j * 128, (j + 1) * 128)
                    for c in range(2):
                        ph.op("pe", lambda e, c=c, s=s, hb=hb, jsl=jsl, qsl=qsl: e.matmul(
                            pS2[s][:, c * 256:(c + 1) * 256], lhsT=dks[hb][:, jsl], rhs=dqs[hb][:, c, qsl],
                            start=(c == 0), stop=False, skip_group_check=True),
                            reads=["dqa" + HB, "dqb" + HB, "dk" + HB], writes=["pS" + S])
                    if j >= 2 * qb:
                        sub = j - 2 * qb
                        for c in range(2):
                            ph.op("pe", lambda e, c=c, s=s, sub=sub: e.matmul(
                                pS2[s][:, c * 256 + sub * 128:c * 256 + (sub + 1) * 128], lhsT=ident_b[:], rhs=mb_b[:],
                                start=False, stop=True, skip_group_check=True), reads=[], writes=["pS" + S])
                    ph.op("act", lambda e, s=s: e.activation(out=PT[s][:], in_=pS2[s][:], func=AF.Exp), reads=["pS" + S], writes=["PT" + S])
                    subs = (0, 1) if j <= 2 * qb else (1,)
                    for sub in subs:
                        for c in range(2):
                            st = first[c]
                            first[c] = False
                            ph.op("pe", lambda e, c=c, s=s, sub=sub, st=st, hb=hb, j=j, ob_=ob_: e.matmul(
                                pO2[c][ob_][:, sub, :], lhsT=PT[s][:, c * 256 + sub * 128:c * 256 + (sub + 1) * 128], rhs=dvp[hb][:, j, :],
                                start=st, stop=False, skip_group_check=True),
                                reads=["PT" + S, "dvp" + HB], writes=[("pO", c, ob_)])
                for sub in range(2):
                    i = 2 * qb + sub
                    tsl = slice(i * 128, (i + 1) * 128)
                    yk = cntY % 2
                    cntY += 1
                    ph.op("dve", lambda e, ob_=ob_, sub=sub: e.reciprocal(out=r01[:, 0:1], in_=pO2[0][ob_][:, sub, 128:129]),
                          reads=[("pO", 0, ob_)], writes=["r0"])
                    ph.op("dve", lambda e, ob_=ob_, sub=sub: e.reciprocal(out=r01[:, 1:2], in_=pO2[1][ob_][:, sub, 128:129]),
                          reads=[("pO", 1, ob_)], writes=["r1"])
                    ph.op("dve", lambda e: e.tensor_tensor(out=r01[:, 1:2], in0=r01[:, 1:2], in1=nlam[:], op=ALU.mult), reads=["r1", "nlam"], writes=["r1"])
                    ph.op("dve", lambda e, ob_=ob_, sub=sub: e.tensor_scalar(out=t1_[:], in0=pO2[1][ob_][:, sub, 0:128], scalar1=r01[:, 1:2], scalar2=None,
                                                                             op0=ALU.mult), reads=[("pO", 1, ob_), "r1"], writes=["t1_"])
                    ph.op("dve", lambda e, ob_=ob_, sub=sub: e.scalar_tensor_tensor(out=o_[:], in0=pO2[0][ob_][:, sub, 0:128], scalar=r01[:, 0:1], in1=t1_[:],
                                                                                    op0=ALU.mult, op1=ALU.add),
                          reads=[("pO", 0, ob_), "r0", "t1_"], writes=["o_"])
                    ph.op("act", lambda e: e.activation(out=junk2[:], in_=o_[:], func=AF.Square, accum_out=ss2[:]), reads=["o_"], writes=["junk2", "ss2"])
                    ph.op("act", lambda e: e.activation(out=ss2[:], in_=ss2[:], func=AF.Sqrt, bias=epsc[:, 0:1], scale=1.0 / 128), reads=["ss2", "epsc"], writes=["ss2"])
                    ph.op("dve", lambda e: e.reciprocal(out=ss2[:], in_=ss2[:]), reads=["ss2"], writes=["ss2"])
                    ph.op("dve", lambda e, yk=yk: e.scalar_tensor_tensor(out=yc[yk][:], in0=o_[:], scalar=ss2[:, 0:1], in1=gout[:], op0=ALU.mult, op1=ALU.mult),
                          reads=["o_", "ss2", "gout"], writes=["yc%d" % yk])
                    ph.op("pe", lambda e, yk=yk: e.transpose(pY2[:], yc[yk][:], ident_b[:]), reads=["yc%d" % yk], writes=["pY"])
                    ph.op("act", lambda e, yk=yk: e.activation(out=ycTt[yk][:], in_=pY2[:], func=AF.Copy), reads=["pY"], writes=["ycT%d" % yk])
                    ph.dma("act", ycT[h * 128:(h + 1) * 128, tsl], ycTt[yk][:], reads=["ycT%d" % yk], writes=[("ycT", h, i)])
        ph.emit()

        ph = Phase(nc, f"b3_{li}")
        ik2 = ph.alloc("ik2", [128, T], F32)
        skp = ph.alloc("skp", [128, 4, T], BF16)
        svp = ph.alloc("svp", [128, NT, 8, 65], BF16)
        iqb = ph.alloc("iqb", [128, 4, 512], F32)
        sqb = ph.alloc("sqb", [128, 8, 512], BF16)
        ph.op("pool", lambda e: e.memset(iqb[:], 0.0), writes=["iqb"])
        ph.op("pool", lambda e: e.memset(sqb[:], 0.0), writes=["sqb"])
        iwb = ph.alloc("iwb", [128, 4, 4], F32)
        accs = [ph.alloc(f"acc{k}", [128, T], F32) for k in range(2)]
        MBT = ph.alloc("MBT", [128, NT, 512], BF16)
        mq01 = ph.alloc("mq01", [128, T], BF16)
        rr_ = [ph.alloc(f"rr{k}", [128, 512], F32) for k in range(2)]
        PT3 = [ph.alloc(f"PT{k}", [128, 512], BF16) for k in range(2)]
        hi = ph.alloc("hi", [128, 1], F32)
        lo = ph.alloc("lo", [128, 1], F32)
        steps = ph.alloc("steps", [128, NBIS + 1], F32)
        pw2 = ph.alloc("pw2", [128, NBIS + 1], F32)
        thr = ph.alloc("thr", [128, 1], F32)
        cntt = ph.alloc("cntt", [128, 1], F32)
        gg = ph.alloc("gg", [128, 1], F32)
        rec = ph.alloc("rec", [128, 4], F32)
        yb = ph.alloc("yb", [128, 4, 512], BF16)
        ybTt = [ph.alloc(f"ybT{k}", [128, 4, 128], BF16) for k in range(2)]
        pI3 = [ph.psum(f"pI{k}", [128, 512], F32) for k in range(2)]
        pM = ph.psum("pM", [128, 4, 128], BF16)
        pS3 = [ph.psum(f"pS{k}", [128, 512], F32) for k in range(2)]
        pO3 = [ph.psum(f"pO{k}", [128, 4, 65], F32) for k in range(2)]
        pY3 = ph.psum("pY", [128, 4, 128], BF16)
        for j in range(NBIS + 1):
            ph.op("pool", lambda e, j=j: e.memset(pw2[:, j:j + 1], 0.5 ** (j + 1)), writes=["pw2"])
        ph.op("pool", lambda e: e.memset(svp[:], 1.0), writes=["svp"])
        ph.op("pool", lambda e: e.memset(MBT[:], -30000.0), writes=["MBT"])
        ph.dma("sp", ik2[0:64, :], ikT, writes=["ik2a"])
        ph.dma("sp", ik2[64:128, :], ikT, writes=["ik2b"])
        ph.dma("sp", skp[:], skT.rearrange("(g p) t -> p g t", p=128), writes=["skp"])
        for n in range(NT):
            ph.dma("sp", svp[:, n, :, 0:64], sv[n * 128:(n + 1) * 128, :].rearrange("t (h v) -> t h v", v=64), reads=["svp"], writes=[("svp", n)])
        NQ4 = T // 512
        cntI = 0
        cntS = 0
        cntO = 0
        cntY = 0
        for QB in range(NQ4):
            qbs = slice(QB * 512, (QB + 1) * 512)
            iq4 = iqT.rearrange("(g two d) t -> two d g t", two=2, d=64)
            sq4 = sqT.rearrange("(g two d) t -> two d g t", two=2, d=64)
            ph.dma("sp", iqb[0:64, 0:4:2, :], iq4[0][:, :, qbs], reads=["iqb"], writes=["iqba"])
            ph.dma("sp", iqb[64:128, 1:4:2, :], iq4[1][:, :, qbs], reads=["iqb"], writes=["iqbb"])
            ph.dma("sp", sqb[0:64, 0:8:2, :], sq4[0][:, :, qbs], reads=["sqb"], writes=["sqba"])
            ph.dma("sp", sqb[64:128, 1:8:2, :], sq4[1][:, :, qbs], reads=["sqb"], writes=["sqbb"])
            ph.dma("sp", iwb[:], iw[qbs, :].rearrange("(s p) h -> p s h", p=128), writes=["iwb"])
            for sub in range(4):
                i = QB * 4 + sub
                a = i % 2
                A = "acc%d" % a
                acc_ = accs[a]
                Tk = (i + 1) * 128
                nkb = (Tk + 511) // 512
                for kb in range(nkb):
                    w = min(512, Tk - kb * 512)
                    ksl = slice(kb * 512, kb * 512 + w)
                    for hh in range(4):
                        p = cntI % 2
                        cntI += 1
                        P = "pI%d" % p
                        half = hh % 2
                        hs = slice(half * 64, (half + 1) * 64)
                        ph.op("pe", lambda e, p=p, w=w, hs=hs, hh=hh, sub=sub, ksl=ksl: e.matmul(
                            pI3[p][:, 0:w], lhsT=iqb[:, hh, sub * 128:(sub + 1) * 128], rhs=ik2[:, ksl], start=True, stop=True),
                            reads=["iqba", "iqbb", "ik2a", "ik2b"], writes=[P])
                        ph.op("act", lambda e, p=p, w=w: e.activation(out=rr_[p][:, 0:w], in_=pI3[p][:, 0:w], func=AF.Relu), reads=[P], writes=["rr%d" % p])
                        if hh == 0:
                            ph.op("dve", lambda e, p=p, w=w, ksl=ksl, sub=sub, acc_=acc_: e.tensor_scalar(
                                out=acc_[:, ksl], in0=rr_[p][:, 0:w], scalar1=iwb[:, sub, 0:1], scalar2=None, op0=ALU.mult),
                                reads=["rr%d" % p, "iwb"], writes=[A])
                        else:
                            ph.op("dve", lambda e, p=p, w=w, ksl=ksl, sub=sub, hh=hh, acc_=acc_: e.scalar_tensor_tensor(
                                out=acc_[:, ksl], in0=rr_[p][:, 0:w], scalar=iwb[:, sub, hh:hh + 1], in1=acc_[:, ksl], op0=ALU.mult, op1=ALU.add),
                                reads=["rr%d" % p, "iwb", A], writes=[A])
                if Tk > TOPK:
                    ph.op("dve", lambda e, acc_=acc_, Tk=Tk: e.tensor_reduce(out=hi[:], in_=acc_[:, 0:Tk], axis=AX.X, op=ALU.max), reads=[A], writes=["hi"])
                    ph.op("dve", lambda e, acc_=acc_, Tk=Tk: e.tensor_reduce(out=lo[:], in_=acc_[:, 0:Tk], axis=AX.X, op=ALU.min), reads=[A], writes=["lo"])
                ph.op("dve", lambda e, acc_=acc_, Tk=Tk: e.tensor_tensor(out=acc_[:, Tk - 128:Tk], in0=acc_[:, Tk - 128:Tk], in1=mbq_f[:], op=ALU.add),
                      reads=[A], writes=[A])
                if Tk > TOPK:
                    ph.op("dve", lambda e: e.tensor_tensor(out=hi[:], in0=hi[:], in1=lo[:], op=ALU.subtract), reads=["hi", "lo"], writes=["hi"])
                    ph.op("dve", lambda e: e.tensor_scalar(out=steps[:], in0=pw2[:], scalar1=hi[:, 0:1], scalar2=None, op0=ALU.mult),
                          reads=["pw2", "hi"], writes=["steps"])
                    ph.op("dve", lambda e: e.tensor_tensor(out=thr[:], in0=lo[:], in1=steps[:, 0:1], op=ALU.add), reads=["lo", "steps"], writes=["thr"])
                    for it in range(NBIS):
                        ph.op("dve", lambda e, acc_=acc_, Tk=Tk: e.tensor_scalar(out=mq01[:, 0:Tk], in0=acc_[:, 0:Tk], scalar1=thr[:, 0:1], scalar2=None,
                                                                                 op0=ALU.is_ge, op1=ALU.add, accum_out=cntt[:]),
                              reads=[A, "thr"], writes=["mq01", "cntt"])
                        ph.op("dve", lambda e, it=it: e.scalar_tensor_tensor(out=gg[:], in0=cntt[:], scalar=float(TOPK), in1=steps[:, it:it + 1],
                                                                             op0=ALU.is_ge, op1=ALU.mult), reads=["cntt", "steps"], writes=["gg"])
                        ph.op("dve", lambda e, it=it: e.scalar_tensor_tensor(out=thr[:], in0=gg[:], scalar=steps[:, it + 1:it + 2], in1=thr[:],
                                                                             op0=ALU.subtract, op1=ALU.add), reads=["gg", "steps", "thr"], writes=["thr"])
                    ph.op("dve", lambda e: e.scalar_tensor_tensor(out=thr[:], in0=steps[:, NBIS:NBIS + 1], scalar=-3.0, in1=thr[:],
                                                                  op0=ALU.mult, op1=ALU.add), reads=["thr", "steps"], writes=["thr"])
                else:
                    ph.op("dve", lambda e: e.memset(thr[:], -1e29), writes=["thr"])
                ph.op("dve", lambda e, acc_=acc_, Tk=Tk: e.tensor_scalar(out=mq01[:, 0:Tk], in0=acc_[:, 0:Tk], scalar1=thr[:, 0:1], scalar2=one_c[:, 0:1],
                                                                         op0=ALU.is_ge, op1=ALU.subtract), reads=[A, "thr"], writes=["mq01"])
                for j0 in range(0, i + 1, 4):
                    nj = min(4, i + 1 - j0)
                    for jj in range(nj):
                        ph.op("pe", lambda e, jj=jj, j0=j0: e.transpose(pM[:, jj, :], mq01[:, (j0 + jj) * 128:(j0 + jj + 1) * 128], ident_b[:]),
                              reads=["mq01"], writes=["pM"])
                    ph.op("act", lambda e, nj=nj, j0=j0, sub=sub: e.activation(out=MBT[:, j0:j0 + nj, sub * 128:(sub + 1) * 128], in_=pM[:, 0:nj, :],
                                                                               func=AF.Copy, scale=30000.0), reads=["pM"], writes=["MBT"])
            njt = QB * 4 + 4
            for h in range(8):
                o = cntO % 2
                cntO += 1
                O = "pO%d" % o
                half = h % 2
                hs = slice(half * 64, (half + 1) * 64)
                firstO = True
                for j in range(njt):
                    s = cntS % 2
                    cntS += 1
                    S = str(s)
                    ph.op("pe", lambda e, s=s, j=j: e.matmul(pS3[s][:], lhsT=ident_b[:], rhs=MBT[:, j, :], start=True, stop=False, skip_group_check=True),
                          reads=["MBT"], writes=["pS" + S])
                    ph.op("pe", lambda e, s=s, j=j, hs=hs, h=h: e.matmul(pS3[s][:], lhsT=skp[:, h // 2, j * 128:(j + 1) * 128], rhs=sqb[:, h, :],
                                                                         start=False, stop=True, skip_group_check=True),
                          reads=["skp", "sqba", "sqbb"], writes=["pS" + S])
                    ph.op("act", lambda e, s=s: e.activation(out=PT3[s][:], in_=pS3[s][:], func=AF.Exp), reads=["pS" + S], writes=["PT" + S])
                    for sub in range(4):
                        if QB * 4 + sub < j:
                            continue
                        st = firstO
                        firstO = False
                        ph.op("pe", lambda e, s=s, sub=sub, o=o, j=j, h=h, st=st: e.matmul(
                            pO3[o][:, sub, :], lhsT=PT3[s][:, sub * 128:(sub + 1) * 128], rhs=svp[:, j, h, :], start=st, stop=False, skip_group_check=True),
                            reads=["PT" + S, ("svp", j)], writes=[O])
                ph.op("dve", lambda e, o=o: e.reciprocal(out=rec[:], in_=pO3[o][:, :, 64]), reads=[O], writes=["rec"])
                ph.op("dve", lambda e, o=o, h=h: e.tensor_tensor(out=yb[:, :, h * 64:(h + 1) * 64], in0=pO3[o][:, :, 0:64],
                                                                 in1=rec[:].unsqueeze(2).to_broadcast([128, 4, 64]), op=ALU.mult),
                      reads=[O, "rec"], writes=["yb"])
            for sub in range(4):
                i = QB * 4 + sub
                yk = cntY % 2
                cntY += 1
                for c in range(4):
                    ph.op("pe", lambda e, c=c, sub=sub: e.transpose(pY3[:, c, :], yb[:, sub, c * 128:(c + 1) * 128], ident_b[:]), reads=["yb"], writes=["pY"])
                ph.op("act", lambda e, yk=yk: e.activation(out=ybTt[yk][:], in_=pY3[:], func=AF.Copy), reads=["pY"], writes=["ybT%d" % yk])
                ph.dma("act", ybT[:, i * 128:(i + 1) * 128].rearrange("(c p) t -> p c t", p=128), ybTt[yk][:], reads=["ybT%d" % yk], writes=[("ybT", i)])
        ph.emit()

        ph = Phase(nc, f"c1_{li}")
        wbr = ph.alloc("wbr", [128, 12, D], BF16)
        wo = ph.alloc("wo", [128, 8, D], BF16)
        wstg = [ph.alloc(f"wstg{k}", [128, 2, D], F32) for k in range(2)]
        for g2_ in range(10):
            g = g2_ // 2
            hf2 = g2_ % 2
            k = g2_ % 2
            K = str(k)
            if g < 3:
                src = w_branch[li, g][hf2 * 256:(hf2 + 1) * 256, :].rearrange("(c p) n -> p c n", p=128)
                dstw = wbr[:, g * 4 + hf2 * 2:g * 4 + hf2 * 2 + 2, :]
            else:
                src = w_out[li][(g - 3) * 512 + hf2 * 256:(g - 3) * 512 + (hf2 + 1) * 256, :].rearrange("(c p) n -> p c n", p=128)
                dstw = wo[:, (g - 3) * 4 + hf2 * 2:(g - 3) * 4 + hf2 * 2 + 2, :]
            ph.dma("sp", wstg[k][:], src, writes=["wstg" + K])
            ph.op("dve" if k == 0 else "pool", lambda e, k=k, dstw=dstw: e.tensor_copy(out=dstw, in_=wstg[k][:]),
                  reads=["wstg" + K] + ([("w", g)] if hf2 else []), writes=[("w", g)])
        yts_ = [ph.alloc(f"yt{b}", [128, 4, 512], BF16) for b in range(3)]
        yts = [[yts_[b], yts_[b]] for b in range(3)]
        gts_ = [ph.alloc(f"gt{b}", [128, 8, 512], BF16) for b in range(3)]
        gts = [[gts_[b], gts_[b]] for b in range(3)]
        mrg = [ph.alloc(f"mrg{k}", [128, 8, 512], BF16) for k in range(2)]
        m1 = [ph.alloc(f"m1{k}", [128, 512], F32) for k in range(3)]
        m2 = ph.alloc("m2s", [128, 512], F32)
        xt2 = [ph.alloc(f"xt{k}", [128, D], F32) for k in range(2)]
        x1t = [ph.alloc(f"x1t{k}", [128, D], F32) for k in range(2)]
        xh2 = [ph.alloc(f"xh{k}", [128, D], BF16) for k in range(2)]
        xT2 = [ph.alloc(f"xT{k}", [128, 8, 128], BF16) for k in range(2)]
        junk4 = ph.alloc("junk4", [128, D], BF16)
        ss4 = ph.alloc("ss4", [128, 1], F32)
        epsc = ph.alloc("epsc", [128, 1], F32)
        ph.op("pool", lambda e: e.memset(epsc[:], EPS), writes=["epsc"])
        pMg = [ph.psum(f"pM{b}", [128, 512], F32) for b in range(3)]
        pX = [ph.psum(f"pX{k}", [128, 512], F32) for k in range(2)]
        pT4 = ph.psum("pT4", [128, 8, 128], BF16)
        ysrc = [yaT, ybT, ycT]
        for tb in range(T // 512):
            k = tb % 2
            K = str(k)
            bsl = slice(tb * 512, (tb + 1) * 512)
            for b in range(3):
                ph.dma("sp", yts[b][k][:], ysrc[b][:, bsl].rearrange("(c p) t -> p c t", p=128), writes=["yt%d" % b])
                ph.dma("sp", gts[b][k][:], gates[b * D:(b + 1) * D, bsl].rearrange("(c p) t -> p c t", p=128), writes=["gt%d" % b])
            for dc in range(8):
                for b in range(3):
                    for c in range(4):
                        ph.op("pe", lambda e, b=b, c=c, dc=dc, k=k: e.matmul(pMg[b][:], lhsT=wbr[:, b * 4 + c, dc * 128:(dc + 1) * 128], rhs=yts[b][k][:, c, :],
                                                                             start=(c == 0), stop=(c == 3)),
                              reads=[("w", b), "yt%d" % b], writes=["pM%d" % b])
                    ph.op("dve", lambda e, b=b, dc=dc, k=k: e.tensor_tensor(out=m1[b][:], in0=pMg[b][:], in1=gts[b][k][:, dc, :], op=ALU.mult),
                          reads=["pM%d" % b, "gt%d" % b], writes=["m1%d" % b])
                ph.op("dve", lambda e: e.tensor_tensor(out=m2[:], in0=m1[0][:], in1=m1[1][:], op=ALU.add), reads=["m10", "m11"], writes=["m2"])
                ph.op("dve", lambda e, dc=dc, k=k: e.tensor_tensor(out=mrg[k][:, dc, :], in0=m2[:], in1=m1[2][:], op=ALU.add),
                      reads=["m2", "m12"], writes=["mrg" + K])
            for tt in range(4):
                i = tb * 4 + tt
                x = i % 2
                X = str(x)
                tsl = slice(i * 128, (i + 1) * 128)
                ph.dma("sp", xt2[x][:], xsrc[tsl, :], writes=["xt" + X])
                for dh in range(2):
                    p = (i * 2 + dh) % 2
                    for c in range(8):
                        ph.op("pe", lambda e, p=p, c=c, k=k, tt=tt, dh=dh: e.matmul(pX[p][:], lhsT=mrg[k][:, c, tt * 128:(tt + 1) * 128],
                                                                                    rhs=wo[:, c, dh * 512:(dh + 1) * 512], start=(c == 0), stop=(c == 7)),
                              reads=["mrg" + K, ("w", 3), ("w", 4)], writes=["pX%d" % p])
                    ph.op("dve", lambda e, p=p, x=x, dh=dh: e.tensor_tensor(out=x1t[x][:, dh * 512:(dh + 1) * 512], in0=pX[p][:],
                                                                            in1=xt2[x][:, dh * 512:(dh + 1) * 512], op=ALU.add),
                          reads=["pX%d" % p, "xt" + X], writes=["x1t" + X])
                ph.dma("act", x1[tsl, :], x1t[x][:], reads=["x1t" + X], writes=[("x1", i)])
                ph.op("act", lambda e, x=x: e.activation(out=junk4[:], in_=x1t[x][:], func=AF.Square, accum_out=ss4[:]), reads=["x1t" + X], writes=["junk4", "ss4"])
                ph.op("act", lambda e: e.activation(out=ss4[:], in_=ss4[:], func=AF.Sqrt, bias=epsc[:, 0:1], scale=1.0 / D), reads=["ss4", "epsc"], writes=["ss4"])
                ph.op("dve", lambda e: e.reciprocal(out=ss4[:], in_=ss4[:]), reads=["ss4"], writes=["ss4"])
                ph.op("dve", lambda e, x=x: e.tensor_scalar(out=xh2[x][:], in0=x1t[x][:], scalar1=ss4[:, 0:1], scalar2=None, op0=ALU.mult),
                      reads=["x1t" + X, "ss4"], writes=["xh" + X])
                for c in range(8):
                    ph.op("pe", lambda e, c=c, x=x: e.transpose(pT4[:, c, :], xh2[x][:, c * 128:(c + 1) * 128], ident_b[:]), reads=["xh" + X], writes=["pT4"])
                ph.op("act", lambda e, x=x: e.activation(out=xT2[x][:], in_=pT4[:], func=AF.Copy), reads=["pT4"], writes=["xT" + X])
                ph.dma("act", xn2T[:, tsl].rearrange("(c p) t -> p c t", p=128), xT2[x][:], reads=["xT" + X], writes=[("xn2T", i)])
        ph.emit()

        ph = Phase(nc, f"c2_{li}")
        NF = FF // 128
        wd = ph.alloc("wd", [128, NF, D], BF16)
        wds = [ph.alloc(f"wds{k}", [128, 2, D], F32) for k in range(2)]
        g2 = ph.alloc("g2", [128, 8], F32)
        load_vec_cols(ph, g2[:], norm_ffn_g[li], "g2")
        for g in range(NF // 2):
            k = g % 2
            ph.dma("sp", wds[k][:], w_down[li][g * 256:(g + 1) * 256, :].rearrange("(c p) n -> p c n", p=128), writes=["wds%d" % k])
            ph.op("dve" if g % 2 == 0 else "pool", lambda e, k=k, g=g: e.tensor_copy(out=wd[:, 2 * g:2 * g + 2, :], in_=wds[k][:]),
                  reads=["wds%d" % k], writes=[("wd", g)])
        TBF = min(1024, T)
        xb = [ph.alloc(f"xb{k}", [128, 8, TBF], BF16) for k in range(2)]
        hT = ph.alloc("hT", [128, NF, TBF], BF16)
        wgs = [ph.alloc(f"wgs{k}", [128, 8, 256], F32) for k in range(2)]
        wg = [ph.alloc(f"wg{k}", [128, 8, 256], BF16) for k in range(2)]
        sg = [ph.alloc(f"sg{k}", [128, 512], F32) for k in range(2)]
        x1b = [ph.alloc(f"x1b{k}", [128, D], F32) for k in range(2)]
        ot = [ph.alloc(f"ot{k}", [128, D], F32) for k in range(2)]
        pG = [ph.psum(f"pG{k}", [128, 512], F32) for k in range(2)]
        pU = [ph.psum(f"pU{k}", [128, 512], F32) for k in range(2)]
        pD = [ph.psum(f"pD{k}", [128, 512], F32) for k in range(2)]
        cntW = 0
        cntG = 0
        for blk in range(T // TBF):
            k = blk % 2
            K = str(k)
            bsl = slice(blk * TBF, (blk + 1) * TBF)
            ph.dma("sp", xb[k][:], xn2T[:, bsl].rearrange("(c p) t -> p c t", p=128), writes=["xb" + K])
            for f in range(NF):
                w = cntW % 2
                cntW += 1
                W = str(w)
                ph.dma("sp", wgs[w][:, :, 0:128], w_gate_up[li][:, f * 128:(f + 1) * 128].rearrange("(c p) n -> p c n", p=128), writes=["wgsa" + W])
                ph.dma("sp", wgs[w][:, :, 128:256], w_gate_up[li][:, FF + f * 128:FF + (f + 1) * 128].rearrange("(c p) n -> p c n", p=128), writes=["wgsb" + W])
                ph.op("pool", lambda e, w=w: e.tensor_tensor(out=wg[w][:], in0=wgs[w][:], in1=g2[:].unsqueeze(2).to_broadcast([128, 8, 256]), op=ALU.mult),
                      reads=["wgsa" + W, "wgsb" + W, "g2"], writes=["wg" + W])
                for hf in range(TBF // 512):
                    q = cntG % 2
                    cntG += 1
                    Q = str(q)
                    hsl = slice(hf * 512, (hf + 1) * 512)
                    for c in range(8):
                        ph.op("pe", lambda e, w=w, q=q, c=c, k=k, hsl=hsl: e.matmul(pG[q][:], lhsT=wg[w][:, c, 0:128], rhs=xb[k][:, c, hsl], start=(c == 0), stop=(c == 7)),
                              reads=["wg" + W, "xb" + K], writes=["pG" + Q])
                    for c in range(8):
                        ph.op("pe", lambda e, w=w, q=q, c=c, k=k, hsl=hsl: e.matmul(pU[q][:], lhsT=wg[w][:, c, 128:256], rhs=xb[k][:, c, hsl], start=(c == 0), stop=(c == 7)),
                              reads=["wg" + W, "xb" + K], writes=["pU" + Q])
                    ph.op("act", lambda e, q=q: e.activation(out=sg[q][:], in_=pG[q][:], func=AF.Silu), reads=["pG" + Q], writes=["sg" + Q])
                    ph.op("dve", lambda e, q=q, f=f, hsl=hsl: e.tensor_tensor(out=hT[:, f, hsl], in0=sg[q][:], in1=pU[q][:], op=ALU.mult),
                          reads=["sg" + Q, "pU" + Q], writes=[("hT", f)])
            for tt in range(TBF // 128):
                i = blk * (TBF // 128) + tt
                x = i % 2
                X = str(x)
                tsl = slice(i * 128, (i + 1) * 128)
                ph.dma("sp", x1b[x][:], x1[tsl, :], writes=["x1b" + X])
                for dh in range(2):
                    p = (i * 2 + dh) % 2
                    for f in range(NF):
                        ph.op("pe", lambda e, p=p, f=f, tt=tt, dh=dh: e.matmul(pD[p][:], lhsT=hT[:, f, tt * 128:(tt + 1) * 128], rhs=wd[:, f, dh * 512:(dh + 1) * 512],
                                                                               start=(f == 0), stop=(f == NF - 1)),
                              reads=[("hT", f), ("wd", f // 2)], writes=["pD%d" % p])
                    ph.op("dve", lambda e, p=p, x=x, dh=dh: e.tensor_tensor(out=ot[x][:, dh * 512:(dh + 1) * 512], in0=pD[p][:],
                                                                            in1=x1b[x][:, dh * 512:(dh + 1) * 512], op=ALU.add),
                          reads=["pD%d" % p, "x1b" + X], writes=["ot" + X])
                ph.dma("act", xdst[tsl, :], ot[x][:], reads=["ot" + X], writes=[("xdst", i)])
        ph.emit()
    top.close()
    return nc


_CACHE = {}


def _rope_tab(T):
    inv = 1.0 / np.power(np.float32(10000.0), np.arange(0, 64, 2, dtype=np.float32) / np.float32(64))
    ang = np.arange(T, dtype=np.float32)[:, None] * inv[None, :].astype(np.float32)
    return np.concatenate([np.cos(ang), np.sin(ang)], axis=1).astype(np.float32)


def kernel(**inputs):
    x = np.asarray(inputs["x"], dtype=np.float32)
    B, T, _ = x.shape
    L = inputs["w_in"].shape[0]
    key = (T, L)
    if key not in _CACHE:
        _CACHE[key] = build(T, L)
    nc = _CACHE[key]
    shared = {}
    for k, v in inputs.items():
        if k == "x":
            continue
        a = np.ascontiguousarray(np.asarray(v, dtype=np.float32))
        if k in ("mlstm_norm_g", "diff_lambda"):
            a = a.reshape(L, -1)
        shared[k] = a
    shared["rope_cs"] = _rope_tab(T)
    in_maps = [dict(shared, x=np.ascontiguousarray(x[b])) for b in range(B)]
    res = run_bass_kernel_spmd(nc, in_maps, core_ids=list(range(B)))
    return np.stack([np.asarray(r["out"]) for r in res.results], axis=0).astype(np.float32)
```
